# Optimizing a Trainium2 kernel written in Bass

```python
import math
import jax, jax.numpy as jnp
from jax import lax
import numpy as np

D_MODEL = 2048
BATCH = 1
SEQ = 8192
DEPTH = 4

N_A_LAYERS = DEPTH // 2
N_B_LAYERS = DEPTH - N_A_LAYERS

MLA_HEADS = 16
Q_LORA = 512
KV_LORA = 512
QK_NOPE = 128
QK_ROPE = 64
V_HEAD = 128
ROPE_THETA = 10000.0
ATTN_Q_BLOCK = 128

MOBA_HEADS = 16
MOBA_HEAD = D_MODEL // MOBA_HEADS
MOBA_BLOCK = 256
MOBA_TOPK = 3
MOBA_Q_CHUNK = 32

REL_BUCKETS = 32
REL_MAX_DIST = 128

D_FF = 5632
CONV_WIDTH = 3

EPS = 1e-6
NEG = -1e30

kernel_name = 'hybrid_mla_moba_yoco'


def rms_norm(x, g):
    xf = x.astype(jnp.float32)
    y = xf * lax.rsqrt(jnp.mean(xf * xf, axis=-1, keepdims=True) + EPS)
    return (y * g.astype(jnp.float32)).astype(x.dtype)


def rope(x, pos):
    half = x.shape[-1] // 2
    inv = ROPE_THETA ** (-jnp.arange(half, dtype=jnp.float32) / half)
    ang = (pos.astype(jnp.float32)[..., None] * inv)[:, :, None, :]
    cos, sin = jnp.cos(ang), jnp.sin(ang)
    x1 = x[..., :half].astype(jnp.float32)
    x2 = x[..., half:].astype(jnp.float32)
    return jnp.concatenate([x1 * cos - x2 * sin, x1 * sin + x2 * cos], axis=-1).astype(x.dtype)


def rel_bucket(dist):
    n = jnp.maximum(dist, 0)
    max_exact = REL_BUCKETS // 2
    nf = jnp.maximum(n, 1).astype(jnp.float32)
    large = max_exact + (jnp.log(nf / max_exact) / math.log(REL_MAX_DIST / max_exact)
                         * (REL_BUCKETS - max_exact)).astype(jnp.int32)
    large = jnp.minimum(large, REL_BUCKETS - 1)
    return jnp.where(n < max_exact, n, large)


def causal_dense_attention(q, k, v, scale):
    B, S, H, Dq = q.shape
    nq = S // ATTN_Q_BLOCK
    qb = q.reshape(B, nq, ATTN_Q_BLOCK, H, Dq).transpose(1, 0, 2, 3, 4)
    k_idx = jnp.arange(S)

    def one_block(args):
        i, qblk = args
        logits = jnp.einsum('bqhd,bkhd->bhqk', qblk, k).astype(jnp.float32) * scale
        q_idx = i * ATTN_Q_BLOCK + jnp.arange(ATTN_Q_BLOCK)
        mask = k_idx[None, :] <= q_idx[:, None]
        logits = jnp.where(mask[None, None], logits, NEG)
        p = jax.nn.softmax(logits, axis=-1).astype(v.dtype)
        return jnp.einsum('bhqk,bkhd->bqhd', p, v)

    out = lax.map(one_block, (jnp.arange(nq), qb))
    return out.transpose(1, 0, 2, 3, 4).reshape(B, S, H, v.shape[-1])


def mla_mixer(xn, pos, w_in, q_norm_g, w_q_up, kv_norm_g, w_kv_up, w_o):
    B, S, _ = xn.shape
    h = xn @ w_in
    c_q, c_kv, k_r = jnp.split(h, [Q_LORA, Q_LORA + KV_LORA], axis=-1)
    q = (rms_norm(c_q, q_norm_g) @ w_q_up).reshape(B, S, MLA_HEADS, QK_NOPE + QK_ROPE)
    q = jnp.concatenate([q[..., :QK_NOPE], rope(q[..., QK_NOPE:], pos)], axis=-1)
    kv = (rms_norm(c_kv, kv_norm_g) @ w_kv_up).reshape(B, S, MLA_HEADS, QK_NOPE + V_HEAD)
    k_nope, v = kv[..., :QK_NOPE], kv[..., QK_NOPE:]
    k_r = rope(k_r[:, :, None, :], pos)
    k = jnp.concatenate([k_nope, jnp.broadcast_to(k_r, (B, S, MLA_HEADS, QK_ROPE))], axis=-1)
    o = causal_dense_attention(q, k, v, (QK_NOPE + QK_ROPE) ** -0.5)
    return o.reshape(B, S, MLA_HEADS * V_HEAD) @ w_o


def shared_moba_kv(h, pos, kv_norm_g, w_kv):
    B, S, _ = h.shape
    kv = rms_norm(h, kv_norm_g) @ w_kv
    k, v = jnp.split(kv, 2, axis=-1)
    nb = -(-S // MOBA_BLOCK)
    pad = nb * MOBA_BLOCK - S
    k = jnp.pad(k, ((0, 0), (0, pad), (0, 0))).reshape(B, nb, MOBA_BLOCK, MOBA_HEADS, MOBA_HEAD)
    v = jnp.pad(v, ((0, 0), (0, pad), (0, 0))).reshape(B, nb, MOBA_BLOCK, MOBA_HEADS, MOBA_HEAD)
    k_mean = jnp.mean(k.astype(jnp.float32), axis=2).astype(k.dtype)
    pos_kb = jnp.pad(pos, ((0, 0), (0, pad))).reshape(B, nb, MOBA_BLOCK)
    return k, v, k_mean, pos_kb


def moba_attention_seq(q, kb, vb, kmean, pos_q, pos_kb, bias_hb):
    S, H, Dk = q.shape
    NB = kb.shape[0]
    topk = min(MOBA_TOPK, NB)
    scale = Dk ** -0.5
    nchunk = S // MOBA_Q_CHUNK
    qc = q.reshape(nchunk, MOBA_Q_CHUNK, H, Dk)
    pc = pos_q.reshape(nchunk, MOBA_Q_CHUNK)
    kbT = kb.transpose(2, 0, 1, 3)
    vbT = vb.transpose(2, 0, 1, 3)
    head_ix = jnp.arange(H)
    blk_ix = jnp.arange(NB)

    def one_chunk(args):
        c, qq, pq = args
        t = c * MOBA_Q_CHUNK + jnp.arange(MOBA_Q_CHUNK)
        own = (c * MOBA_Q_CHUNK) // MOBA_BLOCK
        gate = jnp.einsum('qhd,nhd->qhn', qq, kmean).astype(jnp.float32)
        gate = jnp.where((blk_ix < own)[None, None, :], gate, -jnp.inf)
        _, sel = lax.top_k(gate, topk)
        valid = sel < own
        k_sel = kbT[head_ix[None, :, None], sel]
        v_sel = vbT[head_ix[None, :, None], sel]
        p_sel = pos_kb[sel]
        s_sel = jnp.einsum('qhd,qhnld->qhnl', qq, k_sel).astype(jnp.float32) * scale
        s_sel = s_sel + bias_hb[head_ix[None, :, None, None], rel_bucket(pq[:, None, None, None] - p_sel)]
        s_sel = jnp.where(valid[..., None], s_sel, NEG)
        k_own = lax.dynamic_index_in_dim(kb, own, 0, keepdims=False)
        v_own = lax.dynamic_index_in_dim(vb, own, 0, keepdims=False)
        p_own = lax.dynamic_index_in_dim(pos_kb, own, 0, keepdims=False)
        s_own = jnp.einsum('qhd,lhd->qhl', qq, k_own).astype(jnp.float32) * scale
        s_own = s_own + bias_hb[head_ix[None, :, None], rel_bucket(pq[:, None, None] - p_own[None, None, :])]
        causal = (own * MOBA_BLOCK + jnp.arange(MOBA_BLOCK))[None, :] <= t[:, None]
        s_own = jnp.where(causal[:, None, :], s_own, NEG)
        logits = jnp.concatenate([s_sel.reshape(MOBA_Q_CHUNK, H, topk * MOBA_BLOCK), s_own], axis=-1)
        p = jax.nn.softmax(logits, axis=-1).astype(vb.dtype)
        p_s = p[..., :topk * MOBA_BLOCK].reshape(MOBA_Q_CHUNK, H, topk, MOBA_BLOCK)
        p_o = p[..., topk * MOBA_BLOCK:]
        return (jnp.einsum('qhnl,qhnld->qhd', p_s, v_sel)
                + jnp.einsum('qhl,lhd->qhd', p_o, v_own))

    out = lax.map(one_chunk, (jnp.arange(nchunk), qc, pc))
    return out.reshape(S, H, vb.shape[-1])


def moba_mixer(xn, pos, w_q, w_o, rel_bias, kb, vb, kmean, pos_kb):
    B, S, _ = xn.shape
    q = (xn @ w_q).reshape(B, S, MOBA_HEADS, MOBA_HEAD)
    o = jax.vmap(moba_attention_seq, in_axes=(0, 0, 0, 0, 0, 0, None))(
        q, kb, vb, kmean, pos, pos_kb, rel_bias.T)
    return o.reshape(B, S, MOBA_HEADS * MOBA_HEAD) @ w_o


def conv_glu_ffn(xn, w_in, conv_w, conv_b, w_out):
    S = xn.shape[1]
    h = xn @ w_in
    hp = jnp.pad(h, ((0, 0), (CONV_WIDTH - 1, 0), (0, 0)))
    h = sum(hp[:, j:j + S] * conv_w[j] for j in range(CONV_WIDTH)) + conv_b
    gate, up = jnp.split(h, 2, axis=-1)
    return (jax.nn.gelu(gate, approximate=True) * up) @ w_out


def setup_inputs(seed: int = 0) -> dict:
    key = jax.random.key(seed)
    ks = jax.random.split(key, 20)
    f32 = jnp.float32

    def w(k, shape, fan_in):
        return jax.random.normal(k, shape, f32) * (fan_in ** -0.5)

    def gain(k, shape):
        return 1.0 + 0.02 * jax.random.normal(k, shape, f32)

    x = jax.random.normal(ks[0], (BATCH, SEQ, D_MODEL), f32)
    offs = jax.random.randint(ks[1], (BATCH, 1), 0, 1024, dtype=jnp.int32)
    positions = jnp.arange(SEQ, dtype=jnp.int32)[None, :] + offs
    return {
        'x': x,
        'positions': positions,
        'norm_gains': gain(ks[2], (DEPTH, 4, D_MODEL)),
        'a_w_in': w(ks[3], (N_A_LAYERS, D_MODEL, Q_LORA + KV_LORA + QK_ROPE), D_MODEL),
        'a_q_norm': gain(ks[4], (N_A_LAYERS, Q_LORA)),
        'a_w_q_up': w(ks[5], (N_A_LAYERS, Q_LORA, MLA_HEADS * (QK_NOPE + QK_ROPE)), Q_LORA),
        'a_kv_norm': gain(ks[6], (N_A_LAYERS, KV_LORA)),
        'a_w_kv_up': w(ks[7], (N_A_LAYERS, KV_LORA, MLA_HEADS * (QK_NOPE + V_HEAD)), KV_LORA),
        'a_w_o': w(ks[8], (N_A_LAYERS, MLA_HEADS * V_HEAD, D_MODEL), MLA_HEADS * V_HEAD),
        'b_kv_norm': gain(ks[9], (D_MODEL,)),
        'b_w_kv': w(ks[10], (D_MODEL, 2 * MOBA_HEADS * MOBA_HEAD), D_MODEL),
        'b_w_q': w(ks[11], (N_B_LAYERS, D_MODEL, MOBA_HEADS * MOBA_HEAD), D_MODEL),
        'b_w_o': w(ks[12], (N_B_LAYERS, MOBA_HEADS * MOBA_HEAD, D_MODEL), MOBA_HEADS * MOBA_HEAD),
        'rel_bias': 0.1 * jax.random.normal(ks[13], (REL_BUCKETS, MOBA_HEADS), f32),
        'ffn_w_in': w(ks[14], (DEPTH, D_MODEL, 2 * D_FF), D_MODEL),
        'ffn_conv_w': w(ks[15], (DEPTH, CONV_WIDTH, 2 * D_FF), CONV_WIDTH),
        'ffn_conv_b': 0.02 * jax.random.normal(ks[16], (DEPTH, 2 * D_FF), f32),
        'ffn_w_out': w(ks[17], (DEPTH, D_FF, D_MODEL), D_FF),
    }


def reference(x, positions, norm_gains, a_w_in, a_q_norm, a_w_q_up, a_kv_norm, a_w_kv_up, a_w_o,
              b_kv_norm, b_w_kv, b_w_q, b_w_o, rel_bias, ffn_w_in, ffn_conv_w, ffn_conv_b, ffn_w_out):
    h = x
    shared = None
    for layer in range(DEPTH):
        g = norm_gains[layer]
        if layer == N_A_LAYERS:
            shared = shared_moba_kv(h, positions, b_kv_norm, b_w_kv)
        xn = rms_norm(h, g[0])
        if layer < N_A_LAYERS:
            mix = mla_mixer(xn, positions, a_w_in[layer], a_q_norm[layer], a_w_q_up[layer],
                            a_kv_norm[layer], a_w_kv_up[layer], a_w_o[layer])
        else:
            j = layer - N_A_LAYERS
            kb, vb, kmean, pos_kb = shared
            mix = moba_mixer(xn, positions, b_w_q[j], b_w_o[j], rel_bias, kb, vb, kmean, pos_kb)
        h = h + rms_norm(mix, g[1])
        f = conv_glu_ffn(rms_norm(h, g[2]), ffn_w_in[layer], ffn_conv_w[layer],
                         ffn_conv_b[layer], ffn_w_out[layer])
        h = h + rms_norm(f, g[3])
    return h
```

```python
import math
import numpy as np
import ml_dtypes
from contextlib import ExitStack
import concourse.bass as bass
import concourse.mybir as mybir
from concourse.bass_utils import run_bass_kernel_spmd

F32 = mybir.dt.float32
BF16 = mybir.dt.bfloat16
I32 = mybir.dt.int32
ALU = mybir.AluOpType
AF = mybir.ActivationFunctionType
AX = mybir.AxisListType
NPBF = ml_dtypes.bfloat16

NCORES = 8
S = 8192
D = 2048
TOK = 1024
EPS = 1e-6
DFF = 5632
ENGS = ("pe", "act", "dve", "pool", "sp")


class Buf:
    __slots__ = ("name", "w", "r", "dsem")

    def __init__(self, name=""):
        self.name = name
        self.w = {}
        self.r = {}
        self.dsem = None


class Prog:
    def __init__(self, nc):
        self.nc = nc
        self.es = ExitStack()
        self.semstack = ExitStack()
        self.q = {e: [] for e in ENGS}
        self.sems = {}
        self.cnt = {}
        self.seen = {e: {} for e in ENGS}
        for e in ENGS:
            if e != "sp":
                self._newsem("E_" + e)
        self.nd = 0
        self.ninstr = 0

    def _newsem(self, key):
        self.sems[key] = self.semstack.enter_context(self.nc.semaphore(key))
        self.cnt[key] = 0
        return key

    def sbuf(self, name, shape, dtype):
        return self.es.enter_context(self.nc.sbuf_tensor(name, list(shape), dtype))

    def push_scope(self):
        self._outer = self.es
        self.es = ExitStack()

    def pop_scope(self):
        self.barrier()
        self.es.close()
        self.es = self._outer

    def psum(self, name, shape, dtype=F32):
        return self.es.enter_context(self.nc.psum_tensor(name, list(shape), dtype))

    def _deps(self, reads, writes):
        deps = {}
        for b in reads:
            for k, v in b.w.items():
                if deps.get(k, 0) < v:
                    deps[k] = v
        for b in writes:
            for k, v in b.w.items():
                if deps.get(k, 0) < v:
                    deps[k] = v
            for k, v in b.r.items():
                if deps.get(k, 0) < v:
                    deps[k] = v
        return deps

    def _waits(self, eng, deps):
        seen = self.seen[eng]
        own = "E_" + eng
        for k, v in deps.items():
            if k == own and eng == "pe":
                continue
            if seen.get(k, 0) < v:
                seen[k] = v
                self.q[eng].append(("wait", k, v))

    def _mark(self, key, v, reads, writes):
        for b in writes:
            b.w[key] = v
            b.r = {}
        for b in reads:
            if b.r.get(key, 0) < v:
                b.r[key] = v
        self.ninstr += 1

    def op(self, eng, fn, reads=(), writes=()):
        self._waits(eng, self._deps(reads, writes))
        key = "E_" + eng
        self.cnt[key] += 1
        self.q[eng].append(("op", fn, key, 1))
        self._mark(key, self.cnt[key], reads, writes)

    def dma(self, eng, fn, reads, writes, owner):
        self._waits(eng, self._deps(reads, writes))
        if owner.dsem is None:
            self.nd += 1
            owner.dsem = self._newsem("D%d" % self.nd)
        key = owner.dsem
        self.cnt[key] += 16
        self.q[eng].append(("op", fn, key, 16))
        self._mark(key, self.cnt[key], reads, writes)

    def barrier(self):
        for e in ENGS:
            seen = self.seen[e]
            for k, v in self.cnt.items():
                if v > 0 and seen.get(k, 0) < v:
                    seen[k] = v
                    self.q[e].append(("wait", k, v))

    def finish(self):
        self.barrier()
        engmap = {"pe": "tensor", "act": "scalar", "dve": "vector", "pool": "gpsimd", "sp": "sync"}
        with self.nc.Block() as block:
            for e in ENGS:
                def body(engine, items=self.q[e]):
                    for it in items:
                        if it[0] == "wait":
                            engine.wait_ge(self.sems[it[1]], it[2])
                        else:
                            it[1](engine).then_inc(self.sems[it[2]], it[3])
                getattr(block, engmap[e])(body)
        self.es.close()
        self.semstack.close()


class Slots:
    def __init__(self, P, name, n, shape, dtype, psum=False):
        self.items = []
        for i in range(n):
            t = P.psum(f"{name}{i}", shape, dtype) if psum else P.sbuf(f"{name}{i}", shape, dtype)
            self.items.append((t, Buf(f"{name}{i}")))
        self.i = 0

    def next(self):
        it = self.items[self.i % len(self.items)]
        self.i += 1
        return it


def MM(P, out, lhsT, rhs, start, stop, r, w):
    P.op("pe", lambda e: e.matmul(out, lhsT, rhs, start=start, stop=stop), r, w)


def ACT(P, out, in_, func, r, w, bias=None, scale=None):
    kw = {}
    if bias is not None:
        kw["bias"] = bias
    if scale is not None:
        kw["scale"] = scale
    P.op("act", lambda e: e.activation(out=out, in_=in_, func=func, **kw), r, w)


def TT(P, eng, out, in0, in1, op, r, w):
    P.op(eng, lambda e: e.tensor_tensor(out=out, in0=in0, in1=in1, op=op), r, w)


def TS(P, eng, out, in0, s1, s2, op0, op1, r, w):
    if s2 is None:
        P.op(eng, lambda e: e.tensor_scalar(out=out, in0=in0, scalar1=s1, scalar2=None, op0=op0), r, w)
    else:
        P.op(eng, lambda e: e.tensor_scalar(out=out, in0=in0, scalar1=s1, scalar2=s2, op0=op0, op1=op1), r, w)


def STT(P, eng, out, in0, scalar, in1, op0, op1, r, w):
    eng = "dve"
    P.op(eng, lambda e: e.scalar_tensor_tensor(out=out, in0=in0, scalar=scalar, in1=in1, op0=op0, op1=op1), r, w)


def CP(P, eng, out, in_, r, w):
    if eng == "act":
        P.op("act", lambda e: e.activation(out=out, in_=in_, func=AF.Identity), r, w)
    else:
        P.op(eng, lambda e: e.tensor_copy(out=out, in_=in_), r, w)


def MEMSET(P, eng, out, val, w):
    P.op(eng, lambda e: e.memset(out, val), [], w)


def DMA(P, q, out, in_, r, w, owner):
    P.dma(q, lambda e: e.dma_start(out=out, in_=in_), r, w, owner)


def RSTD(P, ssq_ap, n, dim, tmp, b_ssq, b_tmp):
    ACT(P, tmp, ssq_ap, AF.Sqrt, [b_ssq], [b_tmp], bias=EPS, scale=1.0 / dim)
    P.op("dve", lambda e: e.reciprocal(out=tmp, in_=tmp), [b_tmp], [b_tmp])


def new_nc():
    return bass.Bass("TRN2", target_bir_lowering=False)


def din(nc, name, shape, dt):
    return nc.dram_tensor(name, list(shape), dt, kind="ExternalInput").ap()


def dout(nc, name, shape, dt):
    return nc.dram_tensor(name, list(shape), dt, kind="ExternalOutput").ap()


def dscratch(nc, name, shape, dt):
    return nc.dram_tensor(name, list(shape), dt, kind="Internal").ap()


def setup_common(P):
    ones = P.sbuf("ones", [128, 128], BF16)
    b_ones = Buf("ones")
    MEMSET(P, "pool", ones[:], 1.0, [b_ones])
    return ones, b_ones


def emit_A(P, HT, G, W, OT, ncols, out_dt, ones, b_ones):
    g_sb = P.sbuf("A_g", [128, 16], F32)
    b_g = Buf()
    DMA(P, "sp", g_sb[:], G, [], [b_g], b_g)
    xn = P.sbuf("A_xn", [128, 16, TOK], BF16)
    b_xn = [[Buf() for _ in range(16)] for _ in range(2)]
    hst = Slots(P, "A_h", 2, [128, 16, 512], F32)
    sqs = Slots(P, "A_sq", 2, [128, 512], BF16)
    rts = Slots(P, "A_rt", 2, [128, 512], F32)
    psq = Slots(P, "A_psq", 1, [128, 512], F32, psum=True)
    pso = Slots(P, "A_pso", 4, [128, 512], F32, psum=True)
    HTv = HT.rearrange("(c p) t -> p c t", p=128)
    for p in range(2):
        h, bh = hst.next()
        DMA(P, "sp", h[:], HTv[:, :, p * 512:(p + 1) * 512], [], [bh], bh)
        ssq, bq = psq.next()
        for c in range(16):
            s, bs = sqs.next()
            ACT(P, s[:], h[:, c, :], AF.Square, [bh], [bs])
            MM(P, ssq[:], ones[:], s[:], c == 0, c == 15, [b_ones, bs], [bq])
        rt, brt = rts.next()
        RSTD(P, ssq[:], 512, D, rt[:], bq, brt)
        for c in range(16):
            STT(P, "dve" if c % 2 == 0 else "pool", xn[:, c, p * 512:(p + 1) * 512], h[:, c, :], g_sb[:, c:c + 1], rt[:],
                ALU.mult, ALU.mult, [bh, b_g, brt], [b_xn[p][c]])
    Wv = W.rearrange("(kc p) n -> p kc n", p=128)
    wsl = Slots(P, "A_w", 3, [128, 16, 512], BF16)
    ost = Slots(P, "A_o", 4, [128, 512], out_dt)
    k = 0
    for c0 in range(0, ncols, 512):
        wc = min(512, ncols - c0)
        wt, bw = wsl.next()
        DMA(P, "pool", wt[:, :, 0:wc], Wv[:, :, c0:c0 + wc], [], [bw], bw)
        for p in range(2):
            for j in range(wc // 128):
                ps, bp = pso.next()
                for kc in range(16):
                    MM(P, ps[:], wt[:, kc, j * 128:(j + 1) * 128], xn[:, kc, p * 512:(p + 1) * 512], kc == 0, kc == 15,
                       [bw, b_xn[p][kc]], [bp])
                o, bo = ost.next()
                CP(P, "act" if k % 2 == 0 else "dve", o[:], ps[:], [bp], [bo])
                k += 1
                r0 = c0 + j * 128
                DMA(P, "sp", OT[r0:r0 + 128, p * 512:(p + 1) * 512], o[:], [bo], [], bo)


def build_A(ncols, out_dt):
    nc = new_nc()
    HT = din(nc, "HT", [D, TOK], F32)
    G = din(nc, "G", [128, 16], F32)
    W = din(nc, "W", [D, ncols], F32)
    OT = dout(nc, "OT", [ncols, TOK], out_dt)
    P = Prog(nc)
    ones, b_ones = setup_common(P)
    emit_A(P, HT, G, W, OT, ncols, out_dt, ones, b_ones)
    P.finish()
    return nc


def emit_D(P, OTin, HT, WO, G1, G2, HM, XN2, ones, b_ones):
    g1 = P.sbuf("D_g1", [128, 16], F32)
    g2 = P.sbuf("D_g2", [128, 16], F32)
    b_g1, b_g2 = Buf(), Buf()
    DMA(P, "sp", g1[:], G1, [], [b_g1], b_g1)
    DMA(P, "sp", g2[:], G2, [], [b_g2], b_g2)
    o = P.sbuf("D_o", [128, 16, TOK], BF16)
    b_o = Buf()
    OTv = OTin.rearrange("(c p) t -> p c t", p=128)
    for c in range(16):
        DMA(P, "sp", o[:, c, :], OTv[:, c, :], [], [b_o], b_o)
    mix = P.sbuf("D_mix", [128, 16, TOK], F32)
    b_mix = [[Buf() for _ in range(16)] for _ in range(2)]
    wsl = Slots(P, "D_w", 3, [128, 16, 512], BF16)
    pso = Slots(P, "D_pso", 3, [128, 512], F32, psum=True)
    pss = [P.psum(f"D_pss{i}", [128, 512], F32) for i in range(2)]
    b_pss = [Buf(), Buf()]
    pss2 = [P.psum(f"D_pss2{i}", [128, 512], F32) for i in range(2)]
    b_pss2 = [Buf(), Buf()]
    sqs = Slots(P, "D_sq", 3, [128, 512], BF16)
    WOv = WO.rearrange("(kc p) n -> p kc n", p=128)
    for wg in range(4):
        wt, bw = wsl.next()
        DMA(P, "pool", wt[:], WOv[:, :, wg * 512:(wg + 1) * 512], [], [bw], bw)
        for p in range(2):
            for j in range(4):
                oc = wg * 4 + j
                ps, bp = pso.next()
                for kc in range(16):
                    MM(P, ps[:], wt[:, kc, j * 128:(j + 1) * 128], o[:, kc, p * 512:(p + 1) * 512], kc == 0, kc == 15,
                       [bw, b_o], [bp])
                CP(P, "dve", mix[:, oc, p * 512:(p + 1) * 512], ps[:], [bp], [b_mix[p][oc]])
                s, bs = sqs.next()
                ACT(P, s[:], mix[:, oc, p * 512:(p + 1) * 512], AF.Square, [b_mix[p][oc]], [bs])
                MM(P, pss[p][:], ones[:], s[:], oc == 0, oc == 15, [b_ones, bs], [b_pss[p]])
    import os
    DSTOP = int(os.environ.get("D_STOP", "9"))
    if DSTOP <= 1:
        return
    rt1 = P.sbuf("D_rt1", [128, TOK], F32)
    b_rt1 = [Buf(), Buf()]
    for p in range(2):
        RSTD(P, pss[p][:], 512, D, rt1[:, p * 512:(p + 1) * 512], b_pss[p], b_rt1[p])
    hst = Slots(P, "D_h", 3, [128, TOK], F32)
    HTv = HT.rearrange("(c p) t -> p c t", p=128)
    HMv = HM.rearrange("(c p) t -> p c t", p=128)
    for c in range(16):
        h, bh = hst.next()
        DMA(P, "sp", h[:], HTv[:, c, :], [], [bh], bh)
        for p in range(2):
            sl = slice(p * 512, (p + 1) * 512)
            STT(P, "dve", mix[:, c, sl], mix[:, c, sl], g1[:, c:c + 1], rt1[:, sl], ALU.mult, ALU.mult,
                [b_mix[p][c], b_g1, b_rt1[p]], [b_mix[p][c]])
            TT(P, "pool", mix[:, c, sl], mix[:, c, sl], h[:, sl], ALU.add, [b_mix[p][c], bh], [b_mix[p][c]])
            s, bs = sqs.next()
            ACT(P, s[:], mix[:, c, sl], AF.Square, [b_mix[p][c]], [bs])
            MM(P, pss2[p][:], ones[:], s[:], c == 0, c == 15, [b_ones, bs], [b_pss2[p]])
        DMA(P, "sp", HMv[:, c, :], mix[:, c, :], [b_mix[0][c], b_mix[1][c]], [], bh)
    if DSTOP <= 2:
        return
    rt2 = P.sbuf("D_rt2", [128, TOK], F32)
    b_rt2 = [Buf(), Buf()]
    for p in range(2):
        RSTD(P, pss2[p][:], 512, D, rt2[:, p * 512:(p + 1) * 512], b_pss2[p], b_rt2[p])
    xst = Slots(P, "D_x", 3, [128, TOK], BF16)
    XNv = XN2.rearrange("(c p) t -> p c t", p=128)
    for c in range(16):
        x, bx = xst.next()
        for p in range(2):
            sl = slice(p * 512, (p + 1) * 512)
            STT(P, "dve" if p == 0 else "pool", x[:, sl], mix[:, c, sl], g2[:, c:c + 1], rt2[:, sl], ALU.mult, ALU.mult,
                [b_mix[p][c], b_g2, b_rt2[p]], [bx])
        DMA(P, "sp", XNv[:, c, :], x[:], [bx], [], bx)


def build_D():
    nc = new_nc()
    OTin = din(nc, "OTin", [D, TOK], BF16)
    HT = din(nc, "HT", [D, TOK], F32)
    WO = din(nc, "WO", [D, D], F32)
    G1 = din(nc, "G1", [128, 16], F32)
    G2 = din(nc, "G2", [128, 16], F32)
    HM = dout(nc, "HM", [D, TOK], F32)
    XN2 = dout(nc, "XN2", [D, TOK], BF16)
    P = Prog(nc)
    ones, b_ones = setup_common(P)
    emit_D(P, OTin, HT, WO, G1, G2, HM, XN2, ones, b_ones)
    P.finish()
    return nc


def emit_E(P, XN2H, HM, WIN, CW, CB, WOUT, G3, FT, HOUT, ones, b_ones):
    g3 = P.sbuf("E_g3", [128, 16], F32)
    b_g3 = Buf()
    DMA(P, "sp", g3[:], G3, [], [b_g3], b_g3)
    cw = P.sbuf("E_cw", [128, 88, 3], F32)
    cb = P.sbuf("E_cb", [128, 88], F32)
    b_cw, b_cb = Buf(), Buf()
    DMA(P, "sp", cw[:], CW, [], [b_cw], b_cw)
    DMA(P, "sp", cb[:], CB, [], [b_cb], b_cb)
    NT = TOK + 2
    xn = P.sbuf("E_xn", [128, 16, NT], BF16)
    b_xn = Buf()
    DMA(P, "sp", xn[:], XN2H.rearrange("(c p) t -> p c t", p=128), [], [b_xn], b_xn)
    act = P.sbuf("E_act", [128, 44, TOK], BF16)
    b_act = [Buf() for _ in range(44)]
    wsl = Slots(P, "E_w", 3, [128, 8192], BF16)
    psA = Slots(P, "E_psA", 5, [128, 512], F32, psum=True)
    psC = Slots(P, "E_psC", 1, [128, 512], F32, psum=True)
    pss = [P.psum(f"E_pss{i}", [128, 512], F32) for i in range(2)]
    b_pss = [Buf(), Buf()]
    hss = Slots(P, "E_hs", 2, [128, NT], F32)
    tss = Slots(P, "E_t", 2, [128, TOK], F32)
    gls = Slots(P, "E_gl", 2, [128, TOK], F32)
    WINv = WIN.rearrange("(kc p) n -> p kc n", p=128)
    for g in range(11):
        wg, bwg = wsl.next()
        wgv = wg[:].rearrange("p (k n) -> p k n", n=512)
        DMA(P, "pool", wgv, WINv[:, :, g * 512:(g + 1) * 512], [], [bwg], bwg)
        wu, bwu = wsl.next()
        wuv = wu[:].rearrange("p (k n) -> p k n", n=512)
        DMA(P, "pool", wuv, WINv[:, :, DFF + g * 512:DFF + (g + 1) * 512], [], [bwu], bwu)
        for j in range(4):
            gl_keep = None
            for which in range(2):
                wv, bw = (wgv, bwg) if which == 0 else (wuv, bwu)
                fidx = (g * 4 + j) + (0 if which == 0 else 44)
                hs, bhs = hss.next()
                pa, bpa = psA.next()
                pb, bpb = psA.next()
                pc, bpc = psC.next()
                for (ps, bp, c0, n) in ((pa, bpa, 0, 512), (pb, bpb, 512, 512), (pc, bpc, 1024, 2)):
                    for kc in range(16):
                        MM(P, ps[:, 0:n], wv[:, kc, j * 128:(j + 1) * 128], xn[:, kc, c0:c0 + n], kc == 0, kc == 15,
                           [bw, b_xn], [bp])
                CP(P, "act", hs[:, 0:512], pa[:], [bpa], [bhs])
                CP(P, "act", hs[:, 512:1024], pb[:], [bpb], [bhs])
                CP(P, "act", hs[:, 1024:1026], pc[:, 0:2], [bpc], [bhs])
                t, bt = tss.next()
                TS(P, "dve", t[:], hs[:, 0:TOK], cw[:, fidx, 0:1], cb[:, fidx:fidx + 1], ALU.mult, ALU.add,
                   [bhs, b_cw, b_cb], [bt])
                STT(P, "dve", t[:], hs[:, 1:TOK + 1], cw[:, fidx, 1:2], t[:], ALU.mult, ALU.add, [bhs, b_cw, bt], [bt])
                STT(P, "pool", t[:], hs[:, 2:TOK + 2], cw[:, fidx, 2:3], t[:], ALU.mult, ALU.add, [bhs, b_cw, bt], [bt])
                if which == 0:
                    gl, bgl = gls.next()
                    ACT(P, gl[:], t[:], AF.Gelu_apprx_tanh, [bt], [bgl])
                    gl_keep = (gl, bgl)
                else:
                    gl, bgl = gl_keep
                    kidx = g * 4 + j
                    TT(P, "pool", act[:, kidx, :], gl[:], t[:], ALU.mult, [bgl, bt], [b_act[kidx]])
    WOv = WOUT.rearrange("(kc p) n -> p kc n", p=128)
    fst = Slots(P, "E_f", 2, [128, 512], F32)
    sqs = Slots(P, "E_sq", 3, [128, 512], BF16)
    FTv = FT.rearrange("(c p) t -> p c t", p=128)
    b_ft = [Buf() for _ in range(16)]
    for dc in range(16):
        wo, bwo = wsl.next()
        wov = wo[:, 0:44 * 128].rearrange("p (k n) -> p k n", n=128)
        DMA(P, "pool", wov, WOv[:, :, dc * 128:(dc + 1) * 128], [], [bwo], bwo)
        for p in range(2):
            ps, bp = psA.next()
            for k in range(44):
                MM(P, ps[:], wov[:, k, :], act[:, k, p * 512:(p + 1) * 512], k == 0, k == 43, [bwo, b_act[k]], [bp])
            f, bf = fst.next()
            CP(P, "dve", f[:], ps[:], [bp], [bf])
            s, bs = sqs.next()
            ACT(P, s[:], f[:], AF.Square, [bf], [bs])
            MM(P, pss[p][:], ones[:], s[:], dc == 0, dc == 15, [b_ones, bs], [b_pss[p]])
            DMA(P, "sp", FTv[:, dc, p * 512:(p + 1) * 512], f[:], [bf], [b_ft[dc]], bf)
    rt3 = P.sbuf("E_rt3", [128, TOK], F32)
    b_rt3 = [Buf(), Buf()]
    for p in range(2):
        RSTD(P, pss[p][:], 512, D, rt3[:, p * 512:(p + 1) * 512], b_pss[p], b_rt3[p])
    HMv = HM.rearrange("(c p) t -> p c t", p=128)
    HOv = HOUT.rearrange("(c p) t -> p c t", p=128)
    for c in range(16):
        f, bf = tss.next()
        hm, bhm = gls.next()
        DMA(P, "sp", f[:], FTv[:, c, :], [b_ft[c]], [bf], bf)
        DMA(P, "sp", hm[:], HMv[:, c, :], [], [bhm], bhm)
        for p in range(2):
            sl = slice(p * 512, (p + 1) * 512)
            STT(P, "dve", f[:, sl], f[:, sl], g3[:, c:c + 1], rt3[:, sl], ALU.mult, ALU.mult, [bf, b_g3, b_rt3[p]], [bf])
        TT(P, "pool", f[:], f[:], hm[:], ALU.add, [bf, bhm], [bf])
        DMA(P, "sp", HOv[:, c, :], f[:], [bf], [], bf)


def build_E():
    nc = new_nc()
    XN2H = din(nc, "XN2H", [D, TOK + 2], BF16)
    HM = din(nc, "HM", [D, TOK], F32)
    WIN = din(nc, "WIN", [D, 2 * DFF], F32)
    CW = din(nc, "CW", [128, 88, 3], F32)
    CB = din(nc, "CB", [128, 88], F32)
    WOUT = din(nc, "WOUT", [DFF, D], F32)
    G3 = din(nc, "G3", [128, 16], F32)
    FT = dscratch(nc, "FT", [D, TOK], F32)
    HOUT = dout(nc, "HOUT", [D, TOK], F32)
    P = Prog(nc)
    ones, b_ones = setup_common(P)
    emit_E(P, XN2H, HM, WIN, CW, CB, WOUT, G3, FT, HOUT, ones, b_ones)
    P.finish()
    return nc


def attn_loop(P, hh, scale, KN, bKN, QN, bQN, V, bV, OTh, ones, b_ones, psS, psO, psSUM, pts, rss, ots,
              extra_qk, special):
    pairs = [(qt, kt) for qt in range(16) for kt in range(4 * qt + 4)]
    state = {}
    pend = None

    def emit_pv(qt, kt, pt, bpt):
        if kt == 0:
            state["O"] = psO.next()
            state["SUM"] = psSUM.next()
        O, bO = state["O"]
        SUM, bSUM = state["SUM"]
        last = kt == 4 * qt + 3
        MM(P, O[:], V[:, kt, :], pt[:], kt == 0, last, [bV[kt // 4], bpt], [bO])
        MM(P, SUM[:], ones[:], pt[:], kt == 0, last, [b_ones, bpt], [bSUM])
        if last:
            rs, brs = rss.next()
            P.op("dve", lambda e: e.reciprocal(out=rs[:], in_=SUM[:]), [bSUM], [brs])
            ot, bot = ots.next()
            TT(P, "dve", ot[:], O[:], rs[:], ALU.mult, [bO, brs], [bot])
            DMA(P, "sp", OTh[hh * 128:(hh + 1) * 128, qt * 512:(qt + 1) * 512], ot[:], [bot], [], bot)

    for (qt, kt) in pairs:
        Sp, bS = psS.next()
        MM(P, Sp[:], KN[:, kt * 128:(kt + 1) * 128], QN[:, qt * 512:(qt + 1) * 512], True, False,
           [bKN[kt // 4], bQN[qt]], [bS])
        extra_qk(Sp, bS, qt, kt)
        pt, bpt = pts.next()
        ACT(P, pt[:], Sp[:], AF.Exp, [bS], [bpt], scale=scale)
        sp = special(qt, kt)
        if sp is not None:
            m_ap, m_bufs = sp
            TT(P, "pool", pt[:], pt[:], m_ap, ALU.mult, [bpt] + m_bufs, [bpt])
        if pend is not None:
            emit_pv(*pend)
        pend = (qt, kt, pt, bpt)
    emit_pv(*pend)


C1 = 6.28125
C2 = 2 * math.pi - 6.28125


def emit_rope_tables(P, POS, INV, SGN, ROPE):
    inv = P.sbuf("R_inv", [64, 1], F32)
    sgn = P.sbuf("R_sgn", [64, 1], F32)
    b_inv, b_sgn = Buf(), Buf()
    DMA(P, "sp", inv[:], INV, [], [b_inv], b_inv)
    DMA(P, "sp", sgn[:], SGN, [], [b_sgn], b_sgn)
    pis = Slots(P, "R_pi", 2, [64, 512], I32)
    angs = Slots(P, "R_ang", 2, [64, 512], F32)
    xs = Slots(P, "R_x", 4, [64, 512], F32)
    nfs = Slots(P, "R_nf", 4, [64, 512], F32)
    nis = Slots(P, "R_ni", 4, [64, 512], I32)
    b_rope = [[Buf() for _ in range(16)] for _ in range(2)]
    for tt in range(16):
        cols = slice(tt * 512, (tt + 1) * 512)
        pi, bpi = pis.next()
        DMA(P, "sp", pi[:], POS[:, cols], [], [bpi], bpi)
        ang, bang = angs.next()
        CP(P, "dve", ang[:], pi[:], [bpi], [bang])
        TS(P, "dve", ang[:], ang[:], inv[:, 0:1], None, ALU.mult, None, [bang, b_inv], [bang])
        for which in range(2):
            eng = "dve" if which == 0 else "pool"
            x, bx = xs.next()
            nf, bnf = nfs.next()
            ni, bni = nis.next()
            if which == 0:
                TS(P, eng, x[:], ang[:], float(math.pi / 2), None, ALU.add, None, [bang], [bx])
            else:
                CP(P, eng, x[:], ang[:], [bang], [bx])
            TS(P, eng, nf[:], x[:], float(1.0 / (2 * math.pi)), None, ALU.mult, None, [bx], [bnf])
            CP(P, eng, ni[:], nf[:], [bnf], [bni])
            CP(P, eng, nf[:], ni[:], [bni], [bnf])
            STT(P, eng, x[:], nf[:], -C1, x[:], ALU.mult, ALU.add, [bnf, bx], [bx])
            STT(P, eng, x[:], nf[:], -float(C2), x[:], ALU.mult, ALU.add, [bnf, bx], [bx])
            TS(P, eng, x[:], x[:], float(math.pi), -float(math.pi), ALU.min, ALU.max, [bx], [bx])
            ACT(P, x[:], x[:], AF.Sin, [bx], [bx])
            if which == 1:
                TS(P, eng, x[:], x[:], sgn[:, 0:1], None, ALU.mult, None, [bx, b_sgn], [bx])
            DMA(P, "sp", ROPE[which, :, cols], x[:], [bx], [b_rope[which][tt]], bx)
    return b_rope


def emit_B(P, CT, QG, KG, WQ, WKV, POS, INV, SGN, MASK, ROPE, OTh, ones, b_ones):
    P.push_scope()
    b_rope = emit_rope_tables(P, POS, INV, SGN, ROPE)
    P.pop_scope()
    qg = P.sbuf("B_qg", [128, 4], F32)
    kg = P.sbuf("B_kg", [128, 4], F32)
    b_qg, b_kg = Buf(), Buf()
    DMA(P, "sp", qg[:], QG, [], [b_qg], b_qg)
    DMA(P, "sp", kg[:], KG, [], [b_kg], b_kg)
    wq = P.sbuf("B_wq", [128, 4, 512], BF16)
    wkv = P.sbuf("B_wkv", [128, 4, 512], BF16)
    b_wq, b_wkv = Buf(), Buf()
    DMA(P, "pool", wq[:], WQ.rearrange("(kc p) n -> p kc n", p=128), [], [b_wq], b_wq)
    DMA(P, "pool", wkv[:], WKV.rearrange("(kc p) n -> p kc n", p=128), [], [b_wkv], b_wkv)
    mask = P.sbuf("B_mask", [128, 4, 512], BF16)
    b_mask = Buf()
    DMA(P, "sp", mask[:], MASK, [], [b_mask], b_mask)
    QN = P.sbuf("B_QN", [128, S], BF16)
    QR = P.sbuf("B_QR", [64, S], BF16)
    KN = P.sbuf("B_KN", [128, S], BF16)
    KR = P.sbuf("B_KR", [64, S], BF16)
    V = P.sbuf("B_V", [128, 64, 128], BF16)
    bQN = [Buf() for _ in range(16)]
    bQR = [Buf() for _ in range(16)]
    bKN = [Buf() for _ in range(16)]
    bKR = [Buf() for _ in range(16)]
    bV = [Buf() for _ in range(16)]
    cqs = Slots(P, "B_cq", 2, [128, 4, 512], F32)
    ckvs = Slots(P, "B_ckv", 2, [128, 4, 512], F32)
    krs_ = Slots(P, "B_kr", 2, [64, 2, 512], F32)
    rps = Slots(P, "B_rp", 2, [64, 2, 512], F32)
    cqn = Slots(P, "B_cqn", 2, [128, 4, 512], BF16)
    ckvn = Slots(P, "B_ckvn", 2, [128, 4, 512], BF16)
    sqs = Slots(P, "B_sq", 3, [128, 512], BF16)
    rts = Slots(P, "B_rt", 2, [128, 512], F32)
    tms = Slots(P, "B_tm", 4, [64, 512], F32)
    psM = Slots(P, "B_psM", 3, [128, 512], F32, psum=True)
    psS = Slots(P, "B_psS", 2, [128, 512], F32, psum=True)
    psO = Slots(P, "B_psO", 2, [128, 512], F32, psum=True)
    psSUM = Slots(P, "B_psSUM", 1, [128, 512], F32, psum=True)
    pts = Slots(P, "B_pt", 3, [128, 512], BF16)
    rss = Slots(P, "B_rs", 2, [128, 512], F32)
    ots = Slots(P, "B_ot", 2, [128, 512], BF16)
    CTv = CT[0:1024, :].rearrange("(c p) t -> p c t", p=128)
    scale = float(192 ** -0.5)
    for hh in range(2):
        for tt in range(16):
            cols = slice(tt * 512, (tt + 1) * 512)
            cq, bcq = cqs.next()
            DMA(P, "sp", cq[:], CTv[:, 0:4, cols], [], [bcq], bcq)
            ckv, bckv = ckvs.next()
            DMA(P, "sp", ckv[:], CTv[:, 4:8, cols], [], [bckv], bckv)
            kr, bkr = krs_.next()
            DMA(P, "sp", kr[:], CT[1024:1152, cols].rearrange("(j p) t -> p j t", p=64), [], [bkr], bkr)
            rp, brp = rps.next()
            DMA(P, "sp", rp[:], ROPE[:, :, cols].rearrange("w p t -> p w t"), [b_rope[0][tt], b_rope[1][tt]], [brp], brp)
            outs = []
            for (src, bsrc, gsb, bg, dsts) in ((cq, bcq, qg, b_qg, cqn), (ckv, bckv, kg, b_kg, ckvn)):
                ssq, bq = psM.next()
                for c in range(4):
                    s, bs = sqs.next()
                    ACT(P, s[:], src[:, c, :], AF.Square, [bsrc], [bs])
                    MM(P, ssq[:], ones[:], s[:], c == 0, c == 3, [b_ones, bs], [bq])
                rt, brt = rts.next()
                RSTD(P, ssq[:], 512, 512, rt[:], bq, brt)
                dn, bdn = dsts.next()
                for c in range(4):
                    STT(P, "dve" if c % 2 == 0 else "pool", dn[:, c, :], src[:, c, :], gsb[:, c:c + 1], rt[:],
                        ALU.mult, ALU.mult, [bsrc, bg, brt], [bdn])
                outs.append((dn, bdn))
            (qn_, bqn_), (kvn_, bkvn_) = outs
            h0 = hh * 256
            ps, bp = psM.next()
            for kc in range(4):
                MM(P, ps[:], wq[:, kc, h0:h0 + 128], qn_[:, kc, :], kc == 0, kc == 3, [b_wq, bqn_], [bp])
            CP(P, "act", QN[:, cols], ps[:], [bp], [bQN[tt]])
            pa, bpa = psM.next()
            for kc in range(4):
                MM(P, pa[0:64, :], wq[:, kc, h0 + 128:h0 + 192], qn_[:, kc, :], kc == 0, kc == 3, [b_wq, bqn_], [bpa])
            t1, bt1 = tms.next()
            TT(P, "dve", t1[:], pa[0:64, :], rp[:, 0, :], ALU.mult, [bpa, brp], [bt1])
            pb, bpb = psM.next()
            for kc in range(4):
                MM(P, pb[0:64, :], wq[:, kc, h0 + 192:h0 + 256], qn_[:, kc, :], kc == 0, kc == 3, [b_wq, bqn_], [bpb])
            t2, bt2 = tms.next()
            TT(P, "dve", t2[:], pb[0:64, :], rp[:, 1, :], ALU.mult, [bpb, brp], [bt2])
            TT(P, "pool", QR[:, cols], t1[:], t2[:], ALU.add, [bt1, bt2], [bQR[tt]])
            ps, bp = psM.next()
            for kc in range(4):
                MM(P, ps[:], wkv[:, kc, h0:h0 + 128], kvn_[:, kc, :], kc == 0, kc == 3, [b_wkv, bkvn_], [bp])
            CP(P, "act", KN[:, cols], ps[:], [bp], [bKN[tt]])
            ps, bp = psM.next()
            for k4 in range(4):
                for kc in range(4):
                    MM(P, ps[:, k4 * 128:(k4 + 1) * 128], kvn_[:, kc, k4 * 128:(k4 + 1) * 128], wkv[:, kc, h0 + 128:h0 + 256],
                       kc == 0, kc == 3, [b_wkv, bkvn_], [bp])
            CP(P, "dve", V[:, tt * 4:(tt + 1) * 4, :], ps[:].rearrange("p (a b) -> p a b", b=128), [bp], [bV[tt]])
            t1, bt1 = tms.next()
            TT(P, "pool", t1[:], kr[:, 0, :], rp[:, 0, :], ALU.mult, [bkr, brp], [bt1])
            t2, bt2 = tms.next()
            TT(P, "pool", t2[:], kr[:, 1, :], rp[:, 1, :], ALU.mult, [bkr, brp], [bt2])
            TT(P, "pool", KR[:, cols], t1[:], t2[:], ALU.add, [bt1, bt2], [bKR[tt]])

        def extra_qk(Sp, bS, qt, kt):
            MM(P, Sp[:], KR[:, kt * 128:(kt + 1) * 128], QR[:, qt * 512:(qt + 1) * 512], False, True,
               [bKR[kt // 4], bQR[qt]], [bS])

        def special(qt, kt):
            j = kt - 4 * qt
            if j >= 0:
                return mask[:, j, :], [b_mask]
            return None

        attn_loop(P, hh, scale, KN, bKN, QN, bQN, V, bV, OTh, ones, b_ones, psS, psO, psSUM, pts, rss, ots,
                  extra_qk, special)


def build_B():
    nc = new_nc()
    CT = din(nc, "CT", [1152, S], F32)
    QG = din(nc, "QG", [128, 4], F32)
    KG = din(nc, "KG", [128, 4], F32)
    WQ = din(nc, "WQ", [512, 512], F32)
    WKV = din(nc, "WKV", [512, 512], F32)
    POS = din(nc, "POS", [64, S], I32)
    INV = din(nc, "INV", [64, 1], F32)
    SGN = din(nc, "SGN", [64, 1], F32)
    MASK = din(nc, "MASK", [128, 4, 512], BF16)
    ROPE = dscratch(nc, "ROPE", [2, 64, S], F32)
    OTh = dout(nc, "OTh", [256, S], BF16)
    P = Prog(nc)
    ones, b_ones = setup_common(P)
    emit_B(P, CT, QG, KG, WQ, WKV, POS, INV, SGN, MASK, ROPE, OTh, ones, b_ones)
    P.finish()
    return nc


_PROGS = {}


def get_prog(key, builder):
    if key not in _PROGS:
        _PROGS[key] = builder()
    return _PROGS[key]


def launch(nc, in_maps):
    res = run_bass_kernel_spmd(nc, in_maps, core_ids=list(range(NCORES)))
    return res.results


def gchunk(g):
    return np.ascontiguousarray(np.asarray(g, np.float32).reshape(-1, 128).T)


def causal_masks():
    kp = np.arange(128)[:, None, None]
    j = np.arange(4)[None, :, None]
    qf = np.arange(512)[None, None, :]
    return (128 * j + kp <= qf).astype(np.float32).astype(NPBF)


def c_(a):
    return np.ascontiguousarray(a)


def run_A(HTs, g, W, out_np_dt, out_dt):
    ncols = W.shape[1]
    nc = get_prog(("A", ncols, str(out_dt)), lambda: build_A(ncols, out_dt))
    G = gchunk(g)
    W = c_(W.astype(np.float32))
    outs = launch(nc, [{"HT": HTs[r], "G": G, "W": W} for r in range(NCORES)])
    return [o["OT"] for o in outs]


def run_D(OT_full, HTs, WO, g1, g2):
    nc = get_prog("D", build_D)
    WO = c_(WO)
    G1, G2 = gchunk(g1), gchunk(g2)
    outs = launch(nc, [{"OTin": c_(OT_full[:, r * TOK:(r + 1) * TOK]), "HT": HTs[r], "WO": WO, "G1": G1, "G2": G2}
                       for r in range(NCORES)])
    return [o["HM"] for o in outs], [o["XN2"] for o in outs]


def run_E(XN2s, HMs, WIN, CWc, CBc, WOUT, g3):
    nc = get_prog("E", build_E)
    CW = c_(np.asarray(CWc, np.float32).T.reshape(88, 128, 3).transpose(1, 0, 2))
    CB = c_(np.asarray(CBc, np.float32).reshape(88, 128).T)
    G3 = gchunk(g3)
    WIN, WOUT = c_(WIN), c_(WOUT)
    maps = []
    for r in range(NCORES):
        halo = np.zeros((D, 2), NPBF) if r == 0 else XN2s[r - 1][:, TOK - 2:TOK]
        maps.append({"XN2H": c_(np.concatenate([halo, XN2s[r]], axis=1)), "HM": HMs[r], "WIN": WIN, "CW": CW,
                     "CB": CB, "WOUT": WOUT, "G3": G3})
    outs = launch(nc, maps)
    return [o["HOUT"] for o in outs]


ROPE_PERM = np.concatenate([np.arange(32, 64), np.arange(0, 32)])


DBG = {}


def run_mla_layer(HTs, positions, g, w_in, q_norm, w_q_up, kv_norm, w_kv_up, w_o, ffn):
    w_in = np.asarray(w_in, np.float32)
    Wext = np.concatenate([w_in, w_in[:, 1024 + ROPE_PERM]], axis=1)
    CTs = run_A(HTs, g[0], Wext, np.float32, F32)
    CT = c_(np.concatenate(CTs, axis=1))
    DBG["CT"] = CT
    ncB = get_prog("B", build_B)
    POS = c_(np.broadcast_to(np.asarray(positions, np.int32).reshape(1, S), (64, S)))
    inv = (np.float32(10000.0) ** (-np.arange(32, dtype=np.float32) / np.float32(32))).astype(np.float32)
    INV = c_(np.concatenate([inv, inv]).reshape(64, 1))
    SGN = c_(np.concatenate([-np.ones(32, np.float32), np.ones(32, np.float32)]).reshape(64, 1))
    MASK = causal_masks()
    QG, KG = gchunk(q_norm), gchunk(kv_norm)
    maps = []
    for r in range(NCORES):
        wq, wkv = [], []
        for h in (2 * r, 2 * r + 1):
            b = h * 192
            wq += [w_q_up[:, b:b + 128], w_q_up[:, b + 128:b + 192], w_q_up[:, b + 128 + ROPE_PERM]]
            wkv += [w_kv_up[:, h * 256:(h + 1) * 256]]
        maps.append({"CT": CT, "QG": QG, "KG": KG, "WQ": c_(np.concatenate(wq, axis=1)),
                     "WKV": c_(np.concatenate(wkv, axis=1)), "POS": POS, "INV": INV, "SGN": SGN, "MASK": MASK})
    outs = launch(ncB, maps)
    OT_full = np.concatenate([o["OTh"] for o in outs], axis=0)
    DBG["OT"] = OT_full
    HMs, XN2s = run_D(OT_full, HTs, w_o, g[1], g[2])
    DBG["HM"] = HMs
    DBG["XN2"] = XN2s
    return run_E(XN2s, HMs, *ffn, g[3])


def rel_thresholds():
    n = np.arange(0, 512)
    nf = np.maximum(n, 1).astype(np.float32)
    large = 16 + (np.log(nf / np.float32(16)) / np.float32(math.log(128 / 16)) * np.float32(16)).astype(np.int32)
    large = np.minimum(large, 31)
    bucket = np.where(n < 16, n, large)
    return [int(np.min(n[bucket >= b])) for b in range(1, 32)]


def emit_C(P, QT, KT, VT, RB, POSQ, POSK, MASK, EALL, IDENT, OTh, ones, b_ones):
    thr = rel_thresholds()
    mask = P.sbuf("C_mask", [128, 4, 512], BF16)
    eall = P.sbuf("C_eall", [32, 32 * 128], BF16)
    ident = P.sbuf("C_ident", [128, 128], F32)
    rb = P.sbuf("C_rb", [128, 2, 32], F32)
    b_mask, b_eall, b_ident, b_rb = Buf(), Buf(), Buf(), Buf()
    DMA(P, "sp", mask[:], MASK, [], [b_mask], b_mask)
    DMA(P, "sp", eall[:], EALL, [], [b_eall], b_eall)
    DMA(P, "sp", ident[:], IDENT, [], [b_ident], b_ident)
    DMA(P, "sp", rb[:], RB, [], [b_rb], b_rb)
    MULT = P.sbuf("C_mult", [128, 2, 5, 512], BF16)
    b_mult = Buf()
    P.push_scope()
    dl = P.sbuf("C_dl", [128, 2, 32], F32)
    c0 = P.sbuf("C_c0", [128, 2], F32)
    b_dl, b_c0 = Buf(), Buf()
    TT(P, "dve", dl[:, :, 1:32], rb[:, :, 1:32], rb[:, :, 0:31], ALU.subtract, [b_rb], [b_dl])
    TT(P, "dve", c0[:], rb[:, :, 0], rb[:, :, 31], ALU.subtract, [b_rb], [b_c0])
    pqi = P.sbuf("C_pqi", [128, 512], I32)
    pki = P.sbuf("C_pki", [128, 5], I32)
    pq = P.sbuf("C_pq", [128, 512], F32)
    pk = P.sbuf("C_pk", [128, 5], F32)
    b_pqi, b_pki, b_pq, b_pk = Buf(), Buf(), Buf(), Buf()
    DMA(P, "sp", pqi[:], POSQ, [], [b_pqi], b_pqi)
    DMA(P, "sp", pki[:], POSK, [], [b_pki], b_pki)
    CP(P, "dve", pq[:], pqi[:], [b_pqi], [b_pq])
    CP(P, "dve", pk[:], pki[:], [b_pki], [b_pk])
    dists = Slots(P, "C_dist", 2, [128, 512], F32)
    accs = Slots(P, "C_acc", 2, [128, 512], F32)
    tmps = Slots(P, "C_tmp", 3, [128, 512], F32)
    for j in range(5):
        dist, bd = dists.next()
        TS(P, "dve", dist[:], pq[:], pk[:, j:j + 1], None, ALU.subtract, None, [b_pq, b_pk], [bd])
        for h in range(2):
            acc, ba = accs.next()
            TS(P, "dve", acc[:], dist[:], float(thr[0]), dl[:, h, 1:2], ALU.is_ge, ALU.mult, [bd, b_dl], [ba])
            for b in range(2, 32):
                tmp, bt = tmps.next()
                TS(P, "dve", tmp[:], dist[:], float(thr[b - 1]), dl[:, h, b:b + 1], ALU.is_ge, ALU.mult, [bd, b_dl], [bt])
                TT(P, "pool", acc[:], acc[:], tmp[:], ALU.add, [ba, bt], [ba])
            if j == 0:
                ACT(P, MULT[:, h, j, :], acc[:], AF.Exp, [ba, b_c0], [b_mult], bias=c0[:, h:h + 1], scale=1.0)
            else:
                ACT(P, acc[:], acc[:], AF.Exp, [ba, b_c0], [ba], bias=c0[:, h:h + 1], scale=1.0)
                TT(P, "pool", MULT[:, h, j, :], acc[:], mask[:, j - 1, :], ALU.mult, [ba, b_mask], [b_mult])
    P.pop_scope()
    Q = P.sbuf("C_Q", [128, S], BF16)
    K = P.sbuf("C_K", [128, S], BF16)
    V = P.sbuf("C_V", [128, 64, 128], BF16)
    MBT = P.sbuf("C_MBT", [32, S], BF16)
    bQ = [Buf() for _ in range(16)]
    bK = [Buf() for _ in range(16)]
    bV = [Buf() for _ in range(16)]
    bMB = [Buf() for _ in range(16)]
    kmf = P.sbuf("C_kmf", [128, 32], F32)
    kmT = P.sbuf("C_kmT", [128, 32], BF16)
    b_kmf, b_kmT = Buf(), Buf()
    gs = Slots(P, "C_g", 2, [128, 32], F32)
    mxs = Slots(P, "C_mx", 2, [128, 8], F32)
    mbs = Slots(P, "C_mb", 2, [128, 32], F32)
    psM = Slots(P, "C_psM", 3, [128, 512], F32, psum=True)
    psS = Slots(P, "C_psS", 2, [128, 512], F32, psum=True)
    psO = Slots(P, "C_psO", 2, [128, 512], F32, psum=True)
    psSUM = Slots(P, "C_psSUM", 1, [128, 512], F32, psum=True)
    pts = Slots(P, "C_pt", 3, [128, 512], BF16)
    rss = Slots(P, "C_rs", 2, [128, 512], F32)
    ots = Slots(P, "C_ot", 2, [128, 512], BF16)
    VTv = VT.rearrange("(t p) d -> p t d", p=128)
    scale = float(128 ** -0.5)
    for hh in range(2):
        for tt in range(16):
            cols = slice(tt * 512, (tt + 1) * 512)
            DMA(P, "sp", Q[:, cols], QT[hh * 128:(hh + 1) * 128, cols], [], [bQ[tt]], bQ[tt])
            DMA(P, "sp", K[:, cols], KT[hh * 128:(hh + 1) * 128, cols], [], [bK[tt]], bK[tt])
            DMA(P, "sp", V[:, tt * 4:(tt + 1) * 4, :], VTv[:, tt * 4:(tt + 1) * 4, hh * 128:(hh + 1) * 128], [], [bV[tt]], bV[tt])
        P.op("dve", lambda e: e.tensor_reduce(out=kmf[:], in_=K[:].rearrange("p (n l) -> p n l", l=256), axis=AX.X, op=ALU.add),
             bK, [b_kmf])
        TS(P, "dve", kmT[:], kmf[:], float(1.0 / 256), None, ALU.mult, None, [b_kmf], [b_kmT])
        for i in range(64):
            own = i // 2
            G, bG = psM.next()
            MM(P, G[:, 0:32], Q[:, i * 128:(i + 1) * 128], kmT[:], True, True, [bQ[i // 4], b_kmT], [bG])
            g, bg = gs.next()
            CP(P, "dve", g[:], G[:, 0:32], [bG], [bg])
            MEMSET(P, "dve", g[:, own:32], -1e30, [bg])
            mx, bmx = mxs.next()
            P.op("dve", lambda e, mx=mx, g=g: e.max(out=mx[:], in_=g[:]), [bg], [bmx])
            mb, bmb = mbs.next()
            TS(P, "dve", mb[:], g[:], mx[:, 2:3], -30000.0, ALU.is_lt, ALU.mult, [bg, bmx], [bmb])
            MEMSET(P, "dve", mb[:, own:own + 1], 0.0, [bmb])
            if own + 1 < 32:
                MEMSET(P, "dve", mb[:, own + 1:32], -30000.0, [bmb])
            T, bT = psM.next()
            P.op("pe", lambda e, T=T, mb=mb: e.transpose(T[0:32, 0:128], mb[:], ident[:]), [bmb, b_ident], [bT])
            CP(P, "act", MBT[:, i * 128:(i + 1) * 128], T[0:32, 0:128], [bT], [bMB[i // 4]])

        def extra_qk(Sp, bS, qt, kt):
            n = kt // 2
            MM(P, Sp[:], eall[:, n * 128:(n + 1) * 128], MBT[:, qt * 512:(qt + 1) * 512], False, True,
               [b_eall, bMB[qt]], [bS])

        def special(qt, kt, hh=hh):
            j = kt - 4 * qt
            if j >= -1:
                return MULT[:, hh, j + 1, :], [b_mult]
            return None

        attn_loop(P, hh, scale, K, bK, Q, bQ, V, bV, OTh, ones, b_ones, psS, psO, psSUM, pts, rss, ots,
                  extra_qk, special)


def build_C():
    nc = new_nc()
    QT = din(nc, "QT", [256, S], BF16)
    KT = din(nc, "KT", [256, S], BF16)
    VT = din(nc, "VT", [S, 256], BF16)
    RB = din(nc, "RB", [128, 2, 32], F32)
    POSQ = din(nc, "POSQ", [128, 512], I32)
    POSK = din(nc, "POSK", [128, 5], I32)
    MASK = din(nc, "MASK", [128, 4, 512], BF16)
    EALL = din(nc, "EALL", [32, 32 * 128], BF16)
    IDENT = din(nc, "IDENT", [128, 128], F32)
    OTh = dout(nc, "OTh", [256, S], BF16)
    P = Prog(nc)
    ones, b_ones = setup_common(P)
    emit_C(P, QT, KT, VT, RB, POSQ, POSK, MASK, EALL, IDENT, OTh, ones, b_ones)
    P.finish()
    return nc


def run_moba_layer(HTs, positions, g, w_q, w_o, rel_bias, KTf, VTf, ffn):
    QTs = run_A(HTs, g[0], np.asarray(w_q, np.float32), NPBF, BF16)
    QTf = np.concatenate(QTs, axis=1)
    ncC = get_prog("C", build_C)
    pos = np.asarray(positions, np.int32).reshape(S)
    POSQ = c_(np.broadcast_to(pos[512:1024].reshape(1, 512), (128, 512)))
    POSK = c_(pos[384:1024].reshape(5, 128).T)
    MASK = causal_masks()
    EALL = c_(np.repeat(np.eye(32, dtype=np.float32)[:, :, None], 128, axis=2).reshape(32, 32 * 128).astype(NPBF))
    IDENT = np.eye(128, dtype=np.float32)
    rel_bias = np.asarray(rel_bias, np.float32)
    maps = []
    for r in range(NCORES):
        RB = c_(np.broadcast_to(rel_bias[:, 2 * r:2 * r + 2].T.reshape(1, 2, 32), (128, 2, 32)))
        maps.append({"QT": c_(QTf[r * 256:(r + 1) * 256]), "KT": c_(KTf[r * 256:(r + 1) * 256]),
                     "VT": c_(VTf[r * 256:(r + 1) * 256].T), "RB": RB, "POSQ": POSQ, "POSK": POSK,
                     "MASK": MASK, "EALL": EALL, "IDENT": IDENT})
    outs = launch(ncC, maps)
    OT_full = np.concatenate([o["OTh"] for o in outs], axis=0)
    DBG["OTm"] = OT_full
    HMs, XN2s = run_D(OT_full, HTs, w_o, g[1], g[2])
    return run_E(XN2s, HMs, *ffn, g[3])


def kernel(x, positions, norm_gains, a_w_in, a_q_norm, a_w_q_up, a_kv_norm, a_w_kv_up, a_w_o,
           b_kv_norm, b_w_kv, b_w_q, b_w_o, rel_bias, ffn_w_in, ffn_conv_w, ffn_conv_b, ffn_w_out):
    x = np.asarray(x, np.float32)
    HTs = [c_(x[0, r * TOK:(r + 1) * TOK, :].T) for r in range(NCORES)]
    KTf = VTf = None
    for layer in range(4):
        g = np.asarray(norm_gains[layer], np.float32)
        ffn = (np.asarray(ffn_w_in[layer], np.float32), ffn_conv_w[layer], ffn_conv_b[layer],
               np.asarray(ffn_w_out[layer], np.float32))
        if layer < 2:
            HTs = run_mla_layer(HTs, positions, g, a_w_in[layer], a_q_norm[layer], np.asarray(a_w_q_up[layer], np.float32),
                                a_kv_norm[layer], np.asarray(a_w_kv_up[layer], np.float32),
                                np.asarray(a_w_o[layer], np.float32), ffn)
        else:
            if layer == 2:
                KVs = run_A(HTs, b_kv_norm, np.asarray(b_w_kv, np.float32), NPBF, BF16)
                KV = np.concatenate(KVs, axis=1)
                KTf, VTf = KV[:2048], KV[2048:]
            j = layer - 2
            HTs = run_moba_layer(HTs, positions, g, b_w_q[j], np.asarray(b_w_o[j], np.float32), rel_bias, KTf, VTf, ffn)
    out = np.concatenate([h.T for h in HTs], axis=0).reshape(1, S, D).astype(np.float32)
    return out
```

```python
import math
import numpy as np
import ml_dtypes
from contextlib import ExitStack
import concourse.bass as bass
import concourse.mybir as mybir
from concourse.bass_utils import run_bass_kernel_spmd

F32 = mybir.dt.float32
BF16 = mybir.dt.bfloat16
I32 = mybir.dt.int32
ALU = mybir.AluOpType
AF = mybir.ActivationFunctionType
AX = mybir.AxisListType
NPBF = ml_dtypes.bfloat16

NCORES = 8
S = 8192
D = 2048
TOK = 1024
EPS = 1e-6
DFF = 5632
ENGS = ("pe", "act", "dve", "pool", "sp")


class Buf:
    __slots__ = ("name", "w", "r", "dsem")

    def __init__(self, name=""):
        self.name = name
        self.w = {}
        self.r = {}
        self.dsem = None


class Prog:
    def __init__(self, nc):
        self.nc = nc
        self.es = ExitStack()
        self.semstack = ExitStack()
        self.q = {e: [] for e in ENGS}
        self.sems = {}
        self.cnt = {}
        self.seen = {e: {} for e in ENGS}
        for e in ENGS:
            if e != "sp":
                self._newsem("E_" + e)
        self.nd = 0
        self.ninstr = 0

    def _newsem(self, key):
        self.sems[key] = self.semstack.enter_context(self.nc.semaphore(key))
        self.cnt[key] = 0
        return key

    def sbuf(self, name, shape, dtype):
        return self.es.enter_context(self.nc.sbuf_tensor(name, list(shape), dtype))

    def push_scope(self):
        self._outer = self.es
        self.es = ExitStack()

    def pop_scope(self):
        self.barrier()
        self.es.close()
        self.es = self._outer

    def psum(self, name, shape, dtype=F32):
        return self.es.enter_context(self.nc.psum_tensor(name, list(shape), dtype))

    def _deps(self, reads, writes):
        deps = {}
        for b in reads:
            for k, v in b.w.items():
                if deps.get(k, 0) < v:
                    deps[k] = v
        for b in writes:
            for k, v in b.w.items():
                if deps.get(k, 0) < v:
                    deps[k] = v
            for k, v in b.r.items():
                if deps.get(k, 0) < v:
                    deps[k] = v
        return deps

    def _waits(self, eng, deps):
        seen = self.seen[eng]
        own = "E_" + eng
        for k, v in deps.items():
            if k == own and eng == "pe":
                continue
            if seen.get(k, 0) < v:
                seen[k] = v
                self.q[eng].append(("wait", k, v))

    def _mark(self, key, v, reads, writes):
        for b in writes:
            b.w[key] = v
            b.r = {}
        for b in reads:
            if b.r.get(key, 0) < v:
                b.r[key] = v
        self.ninstr += 1

    def op(self, eng, fn, reads=(), writes=()):
        self._waits(eng, self._deps(reads, writes))
        key = "E_" + eng
        self.cnt[key] += 1
        self.q[eng].append(("op", fn, key, 1))
        self._mark(key, self.cnt[key], reads, writes)

    def dma(self, eng, fn, reads, writes, owner):
        self._waits(eng, self._deps(reads, writes))
        if owner.dsem is None:
            self.nd += 1
            owner.dsem = self._newsem("D%d" % self.nd)
        key = owner.dsem
        self.cnt[key] += 16
        self.q[eng].append(("op", fn, key, 16))
        self._mark(key, self.cnt[key], reads, writes)

    def barrier(self):
        for e in ENGS:
            seen = self.seen[e]
            for k, v in self.cnt.items():
                if v > 0 and seen.get(k, 0) < v:
                    seen[k] = v
                    self.q[e].append(("wait", k, v))

    def finish(self):
        self.barrier()
        engmap = {"pe": "tensor", "act": "scalar", "dve": "vector", "pool": "gpsimd", "sp": "sync"}
        with self.nc.Block() as block:
            for e in ENGS:
                def body(engine, items=self.q[e]):
                    for it in items:
                        if it[0] == "wait":
                            engine.wait_ge(self.sems[it[1]], it[2])
                        else:
                            it[1](engine).then_inc(self.sems[it[2]], it[3])
                getattr(block, engmap[e])(body)
        self.es.close()
        self.semstack.close()


class Slots:
    def __init__(self, P, name, n, shape, dtype, psum=False):
        self.items = []
        for i in range(n):
            t = P.psum(f"{name}{i}", shape, dtype) if psum else P.sbuf(f"{name}{i}", shape, dtype)
            self.items.append((t, Buf(f"{name}{i}")))
        self.i = 0

    def next(self):
        it = self.items[self.i % len(self.items)]
        self.i += 1
        return it


def MM(P, out, lhsT, rhs, start, stop, r, w):
    P.op("pe", lambda e: e.matmul(out, lhsT, rhs, start=start, stop=stop), r, w)


def ACT(P, out, in_, func, r, w, bias=None, scale=None):
    kw = {}
    if bias is not None:
        kw["bias"] = bias
    if scale is not None:
        kw["scale"] = scale
    P.op("act", lambda e: e.activation(out=out, in_=in_, func=func, **kw), r, w)


def TT(P, eng, out, in0, in1, op, r, w):
    P.op(eng, lambda e: e.tensor_tensor(out=out, in0=in0, in1=in1, op=op), r, w)


def TS(P, eng, out, in0, s1, s2, op0, op1, r, w):
    if s2 is None:
        P.op(eng, lambda e: e.tensor_scalar(out=out, in0=in0, scalar1=s1, scalar2=None, op0=op0), r, w)
    else:
        P.op(eng, lambda e: e.tensor_scalar(out=out, in0=in0, scalar1=s1, scalar2=s2, op0=op0, op1=op1), r, w)


def STT(P, eng, out, in0, scalar, in1, op0, op1, r, w):
    eng = "dve"
    P.op(eng, lambda e: e.scalar_tensor_tensor(out=out, in0=in0, scalar=scalar, in1=in1, op0=op0, op1=op1), r, w)


def CP(P, eng, out, in_, r, w):
    if eng == "act":
        P.op("act", lambda e: e.activation(out=out, in_=in_, func=AF.Identity), r, w)
    else:
        P.op(eng, lambda e: e.tensor_copy(out=out, in_=in_), r, w)


def MEMSET(P, eng, out, val, w):
    P.op(eng, lambda e: e.memset(out, val), [], w)


def DMA(P, q, out, in_, r, w, owner):
    P.dma(q, lambda e: e.dma_start(out=out, in_=in_), r, w, owner)


def RSTD(P, ssq_ap, n, dim, tmp, b_ssq, b_tmp):
    ACT(P, tmp, ssq_ap, AF.Sqrt, [b_ssq], [b_tmp], bias=EPS, scale=1.0 / dim)
    P.op("dve", lambda e: e.reciprocal(out=tmp, in_=tmp), [b_tmp], [b_tmp])


def new_nc():
    return bass.Bass("TRN2", target_bir_lowering=False)


def din(nc, name, shape, dt):
    return nc.dram_tensor(name, list(shape), dt, kind="ExternalInput").ap()


def dout(nc, name, shape, dt):
    return nc.dram_tensor(name, list(shape), dt, kind="ExternalOutput").ap()


def dscratch(nc, name, shape, dt):
    return nc.dram_tensor(name, list(shape), dt, kind="Internal").ap()


def setup_common(P):
    ones = P.sbuf("ones", [128, 128], BF16)
    b_ones = Buf("ones")
    MEMSET(P, "pool", ones[:], 1.0, [b_ones])
    return ones, b_ones


def emit_A(P, HT, G, W, OT, ncols, out_dt, ones, b_ones):
    g_sb = P.sbuf("A_g", [128, 16], F32)
    b_g = Buf()
    DMA(P, "sp", g_sb[:], G, [], [b_g], b_g)
    xn = P.sbuf("A_xn", [128, 16, TOK], BF16)
    b_xn = [[Buf() for _ in range(16)] for _ in range(2)]
    hst = Slots(P, "A_h", 2, [128, 16, 512], F32)
    sqs = Slots(P, "A_sq", 2, [128, 512], BF16)
    rts = Slots(P, "A_rt", 2, [128, 512], F32)
    psq = Slots(P, "A_psq", 1, [128, 512], F32, psum=True)
    pso = Slots(P, "A_pso", 4, [128, 512], F32, psum=True)
    HTv = HT.rearrange("(c p) t -> p c t", p=128)
    for p in range(2):
        h, bh = hst.next()
        DMA(P, "sp", h[:], HTv[:, :, p * 512:(p + 1) * 512], [], [bh], bh)
        ssq, bq = psq.next()
        for c in range(16):
            s, bs = sqs.next()
            ACT(P, s[:], h[:, c, :], AF.Square, [bh], [bs])
            MM(P, ssq[:], ones[:], s[:], c == 0, c == 15, [b_ones, bs], [bq])
        rt, brt = rts.next()
        RSTD(P, ssq[:], 512, D, rt[:], bq, brt)
        for c in range(16):
            STT(P, "dve" if c % 2 == 0 else "pool", xn[:, c, p * 512:(p + 1) * 512], h[:, c, :], g_sb[:, c:c + 1], rt[:],
                ALU.mult, ALU.mult, [bh, b_g, brt], [b_xn[p][c]])
    Wv = W.rearrange("(kc p) n -> p kc n", p=128)
    wsl = Slots(P, "A_w", 3, [128, 16, 512], BF16)
    ost = Slots(P, "A_o", 4, [128, 512], out_dt)
    k = 0
    for c0 in range(0, ncols, 512):
        wc = min(512, ncols - c0)
        wt, bw = wsl.next()
        DMA(P, "pool", wt[:, :, 0:wc], Wv[:, :, c0:c0 + wc], [], [bw], bw)
        for p in range(2):
            for j in range(wc // 128):
                ps, bp = pso.next()
                for kc in range(16):
                    MM(P, ps[:], wt[:, kc, j * 128:(j + 1) * 128], xn[:, kc, p * 512:(p + 1) * 512], kc == 0, kc == 15,
                       [bw, b_xn[p][kc]], [bp])
                o, bo = ost.next()
                CP(P, "act" if k % 2 == 0 else "dve", o[:], ps[:], [bp], [bo])
                k += 1
                r0 = c0 + j * 128
                DMA(P, "sp", OT[r0:r0 + 128, p * 512:(p + 1) * 512], o[:], [bo], [], bo)


def build_A(ncols, out_dt):
    nc = new_nc()
    HT = din(nc, "HT", [D, TOK], F32)
    G = din(nc, "G", [128, 16], F32)
    W = din(nc, "W", [D, ncols], F32)
    OT = dout(nc, "OT", [ncols, TOK], out_dt)
    P = Prog(nc)
    ones, b_ones = setup_common(P)
    emit_A(P, HT, G, W, OT, ncols, out_dt, ones, b_ones)
    P.finish()
    return nc


def emit_D(P, OTin, HT, WO, G1, G2, HM, XN2, ones, b_ones):
    g1 = P.sbuf("D_g1", [128, 16], F32)
    g2 = P.sbuf("D_g2", [128, 16], F32)
    b_g1, b_g2 = Buf(), Buf()
    DMA(P, "sp", g1[:], G1, [], [b_g1], b_g1)
    DMA(P, "sp", g2[:], G2, [], [b_g2], b_g2)
    o = P.sbuf("D_o", [128, 16, TOK], BF16)
    b_o = Buf()
    OTv = OTin.rearrange("(c p) t -> p c t", p=128)
    for c in range(16):
        DMA(P, "sp", o[:, c, :], OTv[:, c, :], [], [b_o], b_o)
    mix = P.sbuf("D_mix", [128, 16, TOK], F32)
    b_mix = [[Buf() for _ in range(16)] for _ in range(2)]
    wsl = Slots(P, "D_w", 3, [128, 16, 512], BF16)
    pso = Slots(P, "D_pso", 3, [128, 512], F32, psum=True)
    pss = [P.psum(f"D_pss{i}", [128, 512], F32) for i in range(2)]
    b_pss = [Buf(), Buf()]
    pss2 = [P.psum(f"D_pss2{i}", [128, 512], F32) for i in range(2)]
    b_pss2 = [Buf(), Buf()]
    sqs = Slots(P, "D_sq", 3, [128, 512], BF16)
    WOv = WO.rearrange("(kc p) n -> p kc n", p=128)
    for wg in range(4):
        wt, bw = wsl.next()
        DMA(P, "pool", wt[:], WOv[:, :, wg * 512:(wg + 1) * 512], [], [bw], bw)
        for p in range(2):
            for j in range(4):
                oc = wg * 4 + j
                ps, bp = pso.next()
                for kc in range(16):
                    MM(P, ps[:], wt[:, kc, j * 128:(j + 1) * 128], o[:, kc, p * 512:(p + 1) * 512], kc == 0, kc == 15,
                       [bw, b_o], [bp])
                CP(P, "dve", mix[:, oc, p * 512:(p + 1) * 512], ps[:], [bp], [b_mix[p][oc]])
                s, bs = sqs.next()
                ACT(P, s[:], mix[:, oc, p * 512:(p + 1) * 512], AF.Square, [b_mix[p][oc]], [bs])
                MM(P, pss[p][:], ones[:], s[:], oc == 0, oc == 15, [b_ones, bs], [b_pss[p]])
    import os
    DSTOP = int(os.environ.get("D_STOP", "9"))
    if DSTOP <= 1:
        return
    rt1 = P.sbuf("D_rt1", [128, TOK], F32)
    b_rt1 = [Buf(), Buf()]
    for p in range(2):
        RSTD(P, pss[p][:], 512, D, rt1[:, p * 512:(p + 1) * 512], b_pss[p], b_rt1[p])
    hst = Slots(P, "D_h", 3, [128, TOK], F32)
    HTv = HT.rearrange("(c p) t -> p c t", p=128)
    HMv = HM.rearrange("(c p) t -> p c t", p=128)
    for c in range(16):
        h, bh = hst.next()
        DMA(P, "sp", h[:], HTv[:, c, :], [], [bh], bh)
        for p in range(2):
            sl = slice(p * 512, (p + 1) * 512)
            STT(P, "dve", mix[:, c, sl], mix[:, c, sl], g1[:, c:c + 1], rt1[:, sl], ALU.mult, ALU.mult,
                [b_mix[p][c], b_g1, b_rt1[p]], [b_mix[p][c]])
            TT(P, "pool", mix[:, c, sl], mix[:, c, sl], h[:, sl], ALU.add, [b_mix[p][c], bh], [b_mix[p][c]])
            s, bs = sqs.next()
            ACT(P, s[:], mix[:, c, sl], AF.Square, [b_mix[p][c]], [bs])
            MM(P, pss2[p][:], ones[:], s[:], c == 0, c == 15, [b_ones, bs], [b_pss2[p]])
        DMA(P, "sp", HMv[:, c, :], mix[:, c, :], [b_mix[0][c], b_mix[1][c]], [], bh)
    if DSTOP <= 2:
        return
    rt2 = P.sbuf("D_rt2", [128, TOK], F32)
    b_rt2 = [Buf(), Buf()]
    for p in range(2):
        RSTD(P, pss2[p][:], 512, D, rt2[:, p * 512:(p + 1) * 512], b_pss2[p], b_rt2[p])
    xst = Slots(P, "D_x", 3, [128, TOK], BF16)
    XNv = XN2.rearrange("(c p) t -> p c t", p=128)
    for c in range(16):
        x, bx = xst.next()
        for p in range(2):
            sl = slice(p * 512, (p + 1) * 512)
            STT(P, "dve" if p == 0 else "pool", x[:, sl], mix[:, c, sl], g2[:, c:c + 1], rt2[:, sl], ALU.mult, ALU.mult,
                [b_mix[p][c], b_g2, b_rt2[p]], [bx])
        DMA(P, "sp", XNv[:, c, :], x[:], [bx], [], bx)


def build_D():
    nc = new_nc()
    OTin = din(nc, "OTin", [D, TOK], BF16)
    HT = din(nc, "HT", [D, TOK], F32)
    WO = din(nc, "WO", [D, D], F32)
    G1 = din(nc, "G1", [128, 16], F32)
    G2 = din(nc, "G2", [128, 16], F32)
    HM = dout(nc, "HM", [D, TOK], F32)
    XN2 = dout(nc, "XN2", [D, TOK], BF16)
    P = Prog(nc)
    ones, b_ones = setup_common(P)
    emit_D(P, OTin, HT, WO, G1, G2, HM, XN2, ones, b_ones)
    P.finish()
    return nc


def emit_E(P, XN2H, HM, WIN, CW, CB, WOUT, G3, FT, HOUT, ones, b_ones):
    g3 = P.sbuf("E_g3", [128, 16], F32)
    b_g3 = Buf()
    DMA(P, "sp", g3[:], G3, [], [b_g3], b_g3)
    cw = P.sbuf("E_cw", [128, 88, 3], F32)
    cb = P.sbuf("E_cb", [128, 88], F32)
    b_cw, b_cb = Buf(), Buf()
    DMA(P, "sp", cw[:], CW, [], [b_cw], b_cw)
    DMA(P, "sp", cb[:], CB, [], [b_cb], b_cb)
    NT = TOK + 2
    xn = P.sbuf("E_xn", [128, 16, NT], BF16)
    b_xn = Buf()
    DMA(P, "sp", xn[:], XN2H.rearrange("(c p) t -> p c t", p=128), [], [b_xn], b_xn)
    act = P.sbuf("E_act", [128, 44, TOK], BF16)
    b_act = [Buf() for _ in range(44)]
    wsl = Slots(P, "E_w", 6, [128, 4096], BF16)
    psA = Slots(P, "E_psA", 5, [128, 512], F32, psum=True)
    psC = Slots(P, "E_psC", 1, [128, 512], F32, psum=True)
    pss = [P.psum(f"E_pss{i}", [128, 512], F32) for i in range(2)]
    b_pss = [Buf(), Buf()]
    hss = Slots(P, "E_hs", 2, [128, NT], F32)
    tss = Slots(P, "E_t", 2, [128, TOK], F32)
    gls = Slots(P, "E_gl", 2, [128, TOK], F32)
    WINv = WIN.rearrange("(kc p) n -> p kc n", p=128)
    WOv = WOUT.rearrange("(kc p) n -> p kc n", p=128)
    tiles = []
    for g2 in range(22):
        tiles.append(("in", g2 * 256))
        tiles.append(("in", DFF + g2 * 256))
    for dc in range(16):
        tiles.append(("out", dc, 0))
        tiles.append(("out", dc, 1))
    loaded = {}

    def issue(i):
        if i >= len(tiles):
            return
        w, bw = wsl.next()
        td = tiles[i]
        if td[0] == "in":
            v = w[:].rearrange("p (k n) -> p k n", n=256)
            DMA(P, "pool", v, WINv[:, :, td[1]:td[1] + 256], [], [bw], bw)
        else:
            v = w[:, 0:22 * 128].rearrange("p (k n) -> p k n", n=128)
            DMA(P, "pool", v, WOv[:, td[2] * 22:(td[2] + 1) * 22, td[1] * 128:(td[1] + 1) * 128], [], [bw], bw)
        loaded[i] = (v, bw)

    for i in range(6):
        issue(i)
    for g2 in range(22):
        wgv, bwg = loaded[2 * g2]
        wuv, bwu = loaded[2 * g2 + 1]
        for j in range(2):
            gl_keep = None
            for which in range(2):
                wv, bw = (wgv, bwg) if which == 0 else (wuv, bwu)
                fidx = (g2 * 2 + j) + (0 if which == 0 else 44)
                hs, bhs = hss.next()
                pa, bpa = psA.next()
                pb, bpb = psA.next()
                pc, bpc = psC.next()
                for (ps, bp, c0, n) in ((pa, bpa, 0, 512), (pb, bpb, 512, 512), (pc, bpc, 1024, 2)):
                    for kc in range(16):
                        MM(P, ps[:, 0:n], wv[:, kc, j * 128:(j + 1) * 128], xn[:, kc, c0:c0 + n], kc == 0, kc == 15,
                           [bw, b_xn], [bp])
                CP(P, "act", hs[:, 0:512], pa[:], [bpa], [bhs])
                CP(P, "act", hs[:, 512:1024], pb[:], [bpb], [bhs])
                CP(P, "act", hs[:, 1024:1026], pc[:, 0:2], [bpc], [bhs])
                t, bt = tss.next()
                TS(P, "dve", t[:], hs[:, 0:TOK], cw[:, fidx, 0:1], cb[:, fidx:fidx + 1], ALU.mult, ALU.add,
                   [bhs, b_cw, b_cb], [bt])
                STT(P, "dve", t[:], hs[:, 1:TOK + 1], cw[:, fidx, 1:2], t[:], ALU.mult, ALU.add, [bhs, b_cw, bt], [bt])
                STT(P, "dve", t[:], hs[:, 2:TOK + 2], cw[:, fidx, 2:3], t[:], ALU.mult, ALU.add, [bhs, b_cw, bt], [bt])
                if which == 0:
                    gl, bgl = gls.next()
                    ACT(P, gl[:], t[:], AF.Gelu_apprx_tanh, [bt], [bgl])
                    gl_keep = (gl, bgl)
                else:
                    gl, bgl = gl_keep
                    kidx = g2 * 2 + j
                    TT(P, "dve", act[:, kidx, :], gl[:], t[:], ALU.mult, [bgl, bt], [b_act[kidx]])
        issue(2 * g2 + 6)
        issue(2 * g2 + 7)
    fst = Slots(P, "E_f", 2, [128, 512], F32)
    sqs = Slots(P, "E_sq", 3, [128, 512], BF16)
    FTv = FT.rearrange("(c p) t -> p c t", p=128)
    b_ft = [Buf() for _ in range(16)]
    for dc in range(16):
        base = 44 + 2 * dc
        (w0, b0), (w1, b1) = loaded[base], loaded[base + 1]
        for p in range(2):
            ps, bp = psA.next()
            for k in range(44):
                wv, bw = (w0, b0) if k < 22 else (w1, b1)
                MM(P, ps[:], wv[:, k % 22, :], act[:, k, p * 512:(p + 1) * 512], k == 0, k == 43, [bw, b_act[k]], [bp])
            f, bf = fst.next()
            CP(P, "dve", f[:], ps[:], [bp], [bf])
            s, bs = sqs.next()
            ACT(P, s[:], f[:], AF.Square, [bf], [bs])
            MM(P, pss[p][:], ones[:], s[:], dc == 0, dc == 15, [b_ones, bs], [b_pss[p]])
            DMA(P, "sp", FTv[:, dc, p * 512:(p + 1) * 512], f[:], [bf], [b_ft[dc]], bf)
        issue(base + 6)
        issue(base + 7)
    rt3 = P.sbuf("E_rt3", [128, TOK], F32)
    b_rt3 = [Buf(), Buf()]
    for p in range(2):
        RSTD(P, pss[p][:], 512, D, rt3[:, p * 512:(p + 1) * 512], b_pss[p], b_rt3[p])
    HMv = HM.rearrange("(c p) t -> p c t", p=128)
    HOv = HOUT.rearrange("(c p) t -> p c t", p=128)
    for c in range(16):
        f, bf = tss.next()
        hm, bhm = gls.next()
        DMA(P, "sp", f[:], FTv[:, c, :], [b_ft[c]], [bf], bf)
        DMA(P, "sp", hm[:], HMv[:, c, :], [], [bhm], bhm)
        for p in range(2):
            sl = slice(p * 512, (p + 1) * 512)
            STT(P, "dve", f[:, sl], f[:, sl], g3[:, c:c + 1], rt3[:, sl], ALU.mult, ALU.mult, [bf, b_g3, b_rt3[p]], [bf])
        TT(P, "pool", f[:], f[:], hm[:], ALU.add, [bf, bhm], [bf])
        DMA(P, "sp", HOv[:, c, :], f[:], [bf], [], bf)


def build_E():
    nc = new_nc()
    XN2H = din(nc, "XN2H", [D, TOK + 2], BF16)
    HM = din(nc, "HM", [D, TOK], F32)
    WIN = din(nc, "WIN", [D, 2 * DFF], F32)
    CW = din(nc, "CW", [128, 88, 3], F32)
    CB = din(nc, "CB", [128, 88], F32)
    WOUT = din(nc, "WOUT", [DFF, D], F32)
    G3 = din(nc, "G3", [128, 16], F32)
    FT = dscratch(nc, "FT", [D, TOK], F32)
    HOUT = dout(nc, "HOUT", [D, TOK], F32)
    P = Prog(nc)
    ones, b_ones = setup_common(P)
    emit_E(P, XN2H, HM, WIN, CW, CB, WOUT, G3, FT, HOUT, ones, b_ones)
    P.finish()
    return nc


def attn_loop(P, hh, scale, KN, bKN, QN, bQN, V, bV, OTh, ones, b_ones, psS, psO, psSUM, pts, rss, ots,
              extra_qk, special, accs, hls):
    pairs = [(qt, kt) for qt in range(16) for kt in range(4 * qt + 4)]
    state = {}
    LOOK = 2

    def emit_pv(qt, kt, pt, bpt):
        if kt == 0:
            state["O"] = psO.next()
            state["acc"] = accs.next()
        O, bO = state["O"]
        acc, bacc = state["acc"]
        last = kt == 4 * qt + 3
        MM(P, O[:], V[:, kt, :], pt[:], kt == 0, last, [bV[kt // 4], bpt], [bO])
        if kt == 0:
            CP(P, "dve", acc[:], pt[:], [bpt], [bacc])
        else:
            TT(P, "dve", acc[:], acc[:], pt[:], ALU.add, [bacc, bpt], [bacc])
        if last:
            hi, bhi = hls.next()
            lo, blo = hls.next()
            CP(P, "dve", hi[:], acc[:], [bacc], [bhi])
            TT(P, "dve", lo[:], acc[:], hi[:], ALU.subtract, [bacc, bhi], [blo])
            SUM, bSUM = psSUM.next()
            MM(P, SUM[:], ones[:], hi[:], True, False, [b_ones, bhi], [bSUM])
            MM(P, SUM[:], ones[:], lo[:], False, True, [b_ones, blo], [bSUM])
            rs, brs = rss.next()
            P.op("dve", lambda e: e.reciprocal(out=rs[:], in_=SUM[:]), [bSUM], [brs])
            ot, bot = ots.next()
            TT(P, "dve", ot[:], O[:], rs[:], ALU.mult, [bO, brs], [bot])
            DMA(P, "sp", OTh[hh * 128:(hh + 1) * 128, qt * 512:(qt + 1) * 512], ot[:], [bot], [], bot)

    ring = []
    n = len(pairs)
    for idx in range(n + LOOK):
        if idx < n:
            qt, kt = pairs[idx]
            Sp, bS = psS.next()
            MM(P, Sp[:], KN[:, kt * 128:(kt + 1) * 128], QN[:, qt * 512:(qt + 1) * 512], True, False,
               [bKN[kt // 4], bQN[qt]], [bS])
            extra_qk(Sp, bS, qt, kt)
            pt, bpt = pts.next()
            ACT(P, pt[:], Sp[:], AF.Exp, [bS], [bpt], scale=scale)
            sp = special(qt, kt)
            if sp is not None:
                m_ap, m_bufs = sp
                TT(P, "pool", pt[:], pt[:], m_ap, ALU.mult, [bpt] + m_bufs, [bpt])
            ring.append((qt, kt, pt, bpt))
        if idx >= LOOK:
            emit_pv(*ring[idx - LOOK])


C1 = 6.28125
C2 = 2 * math.pi - 6.28125


def emit_rope_tables(P, POS, INV, SGN, ROPE):
    inv = P.sbuf("R_inv", [64, 1], F32)
    sgn = P.sbuf("R_sgn", [64, 1], F32)
    b_inv, b_sgn = Buf(), Buf()
    DMA(P, "sp", inv[:], INV, [], [b_inv], b_inv)
    DMA(P, "sp", sgn[:], SGN, [], [b_sgn], b_sgn)
    pis = Slots(P, "R_pi", 2, [64, 512], I32)
    angs = Slots(P, "R_ang", 2, [64, 512], F32)
    xs = Slots(P, "R_x", 4, [64, 512], F32)
    nfs = Slots(P, "R_nf", 4, [64, 512], F32)
    nis = Slots(P, "R_ni", 4, [64, 512], I32)
    b_rope = [[Buf() for _ in range(16)] for _ in range(2)]
    for tt in range(16):
        cols = slice(tt * 512, (tt + 1) * 512)
        pi, bpi = pis.next()
        DMA(P, "sp", pi[:], POS[:, cols], [], [bpi], bpi)
        ang, bang = angs.next()
        CP(P, "dve", ang[:], pi[:], [bpi], [bang])
        TS(P, "dve", ang[:], ang[:], inv[:, 0:1], None, ALU.mult, None, [bang, b_inv], [bang])
        for which in range(2):
            eng = "dve" if which == 0 else "pool"
            x, bx = xs.next()
            nf, bnf = nfs.next()
            ni, bni = nis.next()
            if which == 0:
                TS(P, eng, x[:], ang[:], float(math.pi / 2), None, ALU.add, None, [bang], [bx])
            else:
                CP(P, eng, x[:], ang[:], [bang], [bx])
            TS(P, eng, nf[:], x[:], float(1.0 / (2 * math.pi)), None, ALU.mult, None, [bx], [bnf])
            CP(P, eng, ni[:], nf[:], [bnf], [bni])
            CP(P, eng, nf[:], ni[:], [bni], [bnf])
            STT(P, eng, x[:], nf[:], -C1, x[:], ALU.mult, ALU.add, [bnf, bx], [bx])
            STT(P, eng, x[:], nf[:], -float(C2), x[:], ALU.mult, ALU.add, [bnf, bx], [bx])
            TS(P, eng, x[:], x[:], float(math.pi), -float(math.pi), ALU.min, ALU.max, [bx], [bx])
            ACT(P, x[:], x[:], AF.Sin, [bx], [bx])
            if which == 1:
                TS(P, eng, x[:], x[:], sgn[:, 0:1], None, ALU.mult, None, [bx, b_sgn], [bx])
            DMA(P, "sp", ROPE[which, :, cols], x[:], [bx], [b_rope[which][tt]], bx)
    return b_rope


def emit_B(P, CT, QG, KG, WQ, WKV, POS, INV, SGN, MASK, ROPE, OTh, ones, b_ones):
    P.push_scope()
    b_rope = emit_rope_tables(P, POS, INV, SGN, ROPE)
    P.pop_scope()
    qg = P.sbuf("B_qg", [128, 4], F32)
    kg = P.sbuf("B_kg", [128, 4], F32)
    b_qg, b_kg = Buf(), Buf()
    DMA(P, "sp", qg[:], QG, [], [b_qg], b_qg)
    DMA(P, "sp", kg[:], KG, [], [b_kg], b_kg)
    wq = P.sbuf("B_wq", [128, 4, 512], BF16)
    wkv = P.sbuf("B_wkv", [128, 4, 512], BF16)
    b_wq, b_wkv = Buf(), Buf()
    DMA(P, "pool", wq[:], WQ.rearrange("(kc p) n -> p kc n", p=128), [], [b_wq], b_wq)
    DMA(P, "pool", wkv[:], WKV.rearrange("(kc p) n -> p kc n", p=128), [], [b_wkv], b_wkv)
    mask = P.sbuf("B_mask", [128, 4, 512], BF16)
    b_mask = Buf()
    DMA(P, "sp", mask[:], MASK, [], [b_mask], b_mask)
    QN = P.sbuf("B_QN", [128, S], BF16)
    QR = P.sbuf("B_QR", [64, S], BF16)
    KN = P.sbuf("B_KN", [128, S], BF16)
    KR = P.sbuf("B_KR", [64, S], BF16)
    V = P.sbuf("B_V", [128, 64, 128], BF16)
    bQN = [Buf() for _ in range(16)]
    bQR = [Buf() for _ in range(16)]
    bKN = [Buf() for _ in range(16)]
    bKR = [Buf() for _ in range(16)]
    bV = [Buf() for _ in range(16)]
    cqs = Slots(P, "B_cq", 2, [128, 4, 512], F32)
    ckvs = Slots(P, "B_ckv", 2, [128, 4, 512], F32)
    krs_ = Slots(P, "B_kr", 2, [64, 2, 512], F32)
    rps = Slots(P, "B_rp", 2, [64, 2, 512], F32)
    cqn = Slots(P, "B_cqn", 2, [128, 4, 512], BF16)
    ckvn = Slots(P, "B_ckvn", 2, [128, 4, 512], BF16)
    sqs = Slots(P, "B_sq", 3, [128, 512], BF16)
    rts = Slots(P, "B_rt", 2, [128, 512], F32)
    tms = Slots(P, "B_tm", 4, [64, 512], F32)
    psM = Slots(P, "B_psM", 2, [128, 512], F32, psum=True)
    psS = Slots(P, "B_psS", 3, [128, 512], F32, psum=True)
    psO = Slots(P, "B_psO", 2, [128, 512], F32, psum=True)
    psSUM = Slots(P, "B_psSUM", 1, [128, 512], F32, psum=True)
    pts = Slots(P, "B_pt", 5, [128, 512], BF16)
    accs = Slots(P, "B_acc", 2, [128, 512], F32)
    hls = Slots(P, "B_hl", 2, [128, 512], BF16)
    rss = Slots(P, "B_rs", 2, [128, 512], F32)
    ots = Slots(P, "B_ot", 2, [128, 512], BF16)
    CTv = CT[0:1024, :].rearrange("(c p) t -> p c t", p=128)
    scale = float(192 ** -0.5)
    for hh in range(2):
        for tt in range(16):
            cols = slice(tt * 512, (tt + 1) * 512)
            cq, bcq = cqs.next()
            DMA(P, "sp", cq[:], CTv[:, 0:4, cols], [], [bcq], bcq)
            ckv, bckv = ckvs.next()
            DMA(P, "sp", ckv[:], CTv[:, 4:8, cols], [], [bckv], bckv)
            kr, bkr = krs_.next()
            DMA(P, "sp", kr[:], CT[1024:1152, cols].rearrange("(j p) t -> p j t", p=64), [], [bkr], bkr)
            rp, brp = rps.next()
            DMA(P, "sp", rp[:], ROPE[:, :, cols].rearrange("w p t -> p w t"), [b_rope[0][tt], b_rope[1][tt]], [brp], brp)
            outs = []
            for (src, bsrc, gsb, bg, dsts) in ((cq, bcq, qg, b_qg, cqn), (ckv, bckv, kg, b_kg, ckvn)):
                ssq, bq = psM.next()
                for c in range(4):
                    s, bs = sqs.next()
                    ACT(P, s[:], src[:, c, :], AF.Square, [bsrc], [bs])
                    MM(P, ssq[:], ones[:], s[:], c == 0, c == 3, [b_ones, bs], [bq])
                rt, brt = rts.next()
                RSTD(P, ssq[:], 512, 512, rt[:], bq, brt)
                dn, bdn = dsts.next()
                for c in range(4):
                    STT(P, "dve" if c % 2 == 0 else "pool", dn[:, c, :], src[:, c, :], gsb[:, c:c + 1], rt[:],
                        ALU.mult, ALU.mult, [bsrc, bg, brt], [bdn])
                outs.append((dn, bdn))
            (qn_, bqn_), (kvn_, bkvn_) = outs
            h0 = hh * 256
            ps, bp = psM.next()
            for kc in range(4):
                MM(P, ps[:], wq[:, kc, h0:h0 + 128], qn_[:, kc, :], kc == 0, kc == 3, [b_wq, bqn_], [bp])
            CP(P, "act", QN[:, cols], ps[:], [bp], [bQN[tt]])
            pa, bpa = psM.next()
            for kc in range(4):
                MM(P, pa[0:64, :], wq[:, kc, h0 + 128:h0 + 192], qn_[:, kc, :], kc == 0, kc == 3, [b_wq, bqn_], [bpa])
            t1, bt1 = tms.next()
            TT(P, "dve", t1[:], pa[0:64, :], rp[:, 0, :], ALU.mult, [bpa, brp], [bt1])
            pb, bpb = psM.next()
            for kc in range(4):
                MM(P, pb[0:64, :], wq[:, kc, h0 + 192:h0 + 256], qn_[:, kc, :], kc == 0, kc == 3, [b_wq, bqn_], [bpb])
            t2, bt2 = tms.next()
            TT(P, "dve", t2[:], pb[0:64, :], rp[:, 1, :], ALU.mult, [bpb, brp], [bt2])
            TT(P, "pool", QR[:, cols], t1[:], t2[:], ALU.add, [bt1, bt2], [bQR[tt]])
            ps, bp = psM.next()
            for kc in range(4):
                MM(P, ps[:], wkv[:, kc, h0:h0 + 128], kvn_[:, kc, :], kc == 0, kc == 3, [b_wkv, bkvn_], [bp])
            CP(P, "act", KN[:, cols], ps[:], [bp], [bKN[tt]])
            ps, bp = psM.next()
            for k4 in range(4):
                for kc in range(4):
                    MM(P, ps[:, k4 * 128:(k4 + 1) * 128], kvn_[:, kc, k4 * 128:(k4 + 1) * 128], wkv[:, kc, h0 + 128:h0 + 256],
                       kc == 0, kc == 3, [b_wkv, bkvn_], [bp])
            CP(P, "dve", V[:, tt * 4:(tt + 1) * 4, :], ps[:].rearrange("p (a b) -> p a b", b=128), [bp], [bV[tt]])
            t1, bt1 = tms.next()
            TT(P, "pool", t1[:], kr[:, 0, :], rp[:, 0, :], ALU.mult, [bkr, brp], [bt1])
            t2, bt2 = tms.next()
            TT(P, "pool", t2[:], kr[:, 1, :], rp[:, 1, :], ALU.mult, [bkr, brp], [bt2])
            TT(P, "pool", KR[:, cols], t1[:], t2[:], ALU.add, [bt1, bt2], [bKR[tt]])

        def extra_qk(Sp, bS, qt, kt):
            MM(P, Sp[:], KR[:, kt * 128:(kt + 1) * 128], QR[:, qt * 512:(qt + 1) * 512], False, True,
               [bKR[kt // 4], bQR[qt]], [bS])

        def special(qt, kt):
            j = kt - 4 * qt
            if j >= 0:
                return mask[:, j, :], [b_mask]
            return None

        attn_loop(P, hh, scale, KN, bKN, QN, bQN, V, bV, OTh, ones, b_ones, psS, psO, psSUM, pts, rss, ots,
                  extra_qk, special, accs, hls)


def build_B():
    nc = new_nc()
    CT = din(nc, "CT", [1152, S], F32)
    QG = din(nc, "QG", [128, 4], F32)
    KG = din(nc, "KG", [128, 4], F32)
    WQ = din(nc, "WQ", [512, 512], F32)
    WKV = din(nc, "WKV", [512, 512], F32)
    POS = din(nc, "POS", [64, S], I32)
    INV = din(nc, "INV", [64, 1], F32)
    SGN = din(nc, "SGN", [64, 1], F32)
    MASK = din(nc, "MASK", [128, 4, 512], BF16)
    ROPE = dscratch(nc, "ROPE", [2, 64, S], F32)
    OTh = dout(nc, "OTh", [256, S], BF16)
    P = Prog(nc)
    ones, b_ones = setup_common(P)
    emit_B(P, CT, QG, KG, WQ, WKV, POS, INV, SGN, MASK, ROPE, OTh, ones, b_ones)
    P.finish()
    return nc


_PROGS = {}


def get_prog(key, builder):
    if key not in _PROGS:
        _PROGS[key] = builder()
    return _PROGS[key]


def launch(nc, in_maps):
    res = run_bass_kernel_spmd(nc, in_maps, core_ids=list(range(NCORES)))
    return res.results


def gchunk(g):
    return np.ascontiguousarray(np.asarray(g, np.float32).reshape(-1, 128).T)


def causal_masks():
    kp = np.arange(128)[:, None, None]
    j = np.arange(4)[None, :, None]
    qf = np.arange(512)[None, None, :]
    return (128 * j + kp <= qf).astype(np.float32).astype(NPBF)


def c_(a):
    return np.ascontiguousarray(a)


def run_A(HTs, g, W, out_np_dt, out_dt):
    ncols = W.shape[1]
    nc = get_prog(("A", ncols, str(out_dt)), lambda: build_A(ncols, out_dt))
    G = gchunk(g)
    W = c_(W.astype(np.float32))
    outs = launch(nc, [{"HT": HTs[r], "G": G, "W": W} for r in range(NCORES)])
    return [o["OT"] for o in outs]


def run_D(OT_full, HTs, WO, g1, g2):
    nc = get_prog("D", build_D)
    WO = c_(WO)
    G1, G2 = gchunk(g1), gchunk(g2)
    outs = launch(nc, [{"OTin": c_(OT_full[:, r * TOK:(r + 1) * TOK]), "HT": HTs[r], "WO": WO, "G1": G1, "G2": G2}
                       for r in range(NCORES)])
    return [o["HM"] for o in outs], [o["XN2"] for o in outs]


def run_E(XN2s, HMs, WIN, CWc, CBc, WOUT, g3):
    nc = get_prog("E", build_E)
    CW = c_(np.asarray(CWc, np.float32).T.reshape(88, 128, 3).transpose(1, 0, 2))
    CB = c_(np.asarray(CBc, np.float32).reshape(88, 128).T)
    G3 = gchunk(g3)
    WIN, WOUT = c_(WIN), c_(WOUT)
    maps = []
    for r in range(NCORES):
        halo = np.zeros((D, 2), NPBF) if r == 0 else XN2s[r - 1][:, TOK - 2:TOK]
        maps.append({"XN2H": c_(np.concatenate([halo, XN2s[r]], axis=1)), "HM": HMs[r], "WIN": WIN, "CW": CW,
                     "CB": CB, "WOUT": WOUT, "G3": G3})
    outs = launch(nc, maps)
    return [o["HOUT"] for o in outs]


ROPE_PERM = np.concatenate([np.arange(32, 64), np.arange(0, 32)])


DBG = {}


def run_mla_layer(HTs, positions, g, w_in, q_norm, w_q_up, kv_norm, w_kv_up, w_o, ffn):
    w_in = np.asarray(w_in, np.float32)
    Wext = np.concatenate([w_in, w_in[:, 1024 + ROPE_PERM]], axis=1)
    CTs = run_A(HTs, g[0], Wext, np.float32, F32)
    CT = c_(np.concatenate(CTs, axis=1))
    DBG["CT"] = CT
    ncB = get_prog("B", build_B)
    POS = c_(np.broadcast_to(np.asarray(positions, np.int32).reshape(1, S), (64, S)))
    inv = (np.float32(10000.0) ** (-np.arange(32, dtype=np.float32) / np.float32(32))).astype(np.float32)
    INV = c_(np.concatenate([inv, inv]).reshape(64, 1))
    SGN = c_(np.concatenate([-np.ones(32, np.float32), np.ones(32, np.float32)]).reshape(64, 1))
    MASK = causal_masks()
    QG, KG = gchunk(q_norm), gchunk(kv_norm)
    maps = []
    for r in range(NCORES):
        wq, wkv = [], []
        for h in (2 * r, 2 * r + 1):
            b = h * 192
            wq += [w_q_up[:, b:b + 128], w_q_up[:, b + 128:b + 192], w_q_up[:, b + 128 + ROPE_PERM]]
            wkv += [w_kv_up[:, h * 256:(h + 1) * 256]]
        maps.append({"CT": CT, "QG": QG, "KG": KG, "WQ": c_(np.concatenate(wq, axis=1)),
                     "WKV": c_(np.concatenate(wkv, axis=1)), "POS": POS, "INV": INV, "SGN": SGN, "MASK": MASK})
    outs = launch(ncB, maps)
    OT_full = np.concatenate([o["OTh"] for o in outs], axis=0)
    DBG["OT"] = OT_full
    HMs, XN2s = run_D(OT_full, HTs, w_o, g[1], g[2])
    DBG["HM"] = HMs
    DBG["XN2"] = XN2s
    return run_E(XN2s, HMs, *ffn, g[3])


def rel_thresholds():
    n = np.arange(0, 512)
    nf = np.maximum(n, 1).astype(np.float32)
    large = 16 + (np.log(nf / np.float32(16)) / np.float32(math.log(128 / 16)) * np.float32(16)).astype(np.int32)
    large = np.minimum(large, 31)
    bucket = np.where(n < 16, n, large)
    return [int(np.min(n[bucket >= b])) for b in range(1, 32)]


def emit_C(P, QT, KT, VT, RB, POSQ, POSK, MASK, EALL, IDENT, OTh, ones, b_ones):
    thr = rel_thresholds()
    mask = P.sbuf("C_mask", [128, 4, 512], BF16)
    eall = P.sbuf("C_eall", [32, 32 * 128], BF16)
    ident = P.sbuf("C_ident", [128, 128], F32)
    rb = P.sbuf("C_rb", [128, 2, 32], F32)
    b_mask, b_eall, b_ident, b_rb = Buf(), Buf(), Buf(), Buf()
    DMA(P, "sp", mask[:], MASK, [], [b_mask], b_mask)
    DMA(P, "sp", eall[:], EALL, [], [b_eall], b_eall)
    DMA(P, "sp", ident[:], IDENT, [], [b_ident], b_ident)
    DMA(P, "sp", rb[:], RB, [], [b_rb], b_rb)
    MULT = P.sbuf("C_mult", [128, 2, 5, 512], BF16)
    b_mult = Buf()
    P.push_scope()
    dl = P.sbuf("C_dl", [128, 2, 32], F32)
    c0 = P.sbuf("C_c0", [128, 2], F32)
    b_dl, b_c0 = Buf(), Buf()
    TT(P, "dve", dl[:, :, 1:32], rb[:, :, 1:32], rb[:, :, 0:31], ALU.subtract, [b_rb], [b_dl])
    TT(P, "dve", c0[:], rb[:, :, 0], rb[:, :, 31], ALU.subtract, [b_rb], [b_c0])
    pqi = P.sbuf("C_pqi", [128, 512], I32)
    pki = P.sbuf("C_pki", [128, 5], I32)
    pq = P.sbuf("C_pq", [128, 512], F32)
    pk = P.sbuf("C_pk", [128, 5], F32)
    b_pqi, b_pki, b_pq, b_pk = Buf(), Buf(), Buf(), Buf()
    DMA(P, "sp", pqi[:], POSQ, [], [b_pqi], b_pqi)
    DMA(P, "sp", pki[:], POSK, [], [b_pki], b_pki)
    CP(P, "dve", pq[:], pqi[:], [b_pqi], [b_pq])
    CP(P, "dve", pk[:], pki[:], [b_pki], [b_pk])
    dists = Slots(P, "C_dist", 2, [128, 512], F32)
    accs = Slots(P, "C_acc", 2, [128, 512], F32)
    tmps = Slots(P, "C_tmp", 3, [128, 512], F32)
    for j in range(5):
        dist, bd = dists.next()
        TS(P, "dve", dist[:], pq[:], pk[:, j:j + 1], None, ALU.subtract, None, [b_pq, b_pk], [bd])
        for h in range(2):
            acc, ba = accs.next()
            TS(P, "dve", acc[:], dist[:], float(thr[0]), dl[:, h, 1:2], ALU.is_ge, ALU.mult, [bd, b_dl], [ba])
            for b in range(2, 32):
                tmp, bt = tmps.next()
                TS(P, "dve", tmp[:], dist[:], float(thr[b - 1]), dl[:, h, b:b + 1], ALU.is_ge, ALU.mult, [bd, b_dl], [bt])
                TT(P, "pool", acc[:], acc[:], tmp[:], ALU.add, [ba, bt], [ba])
            if j == 0:
                ACT(P, MULT[:, h, j, :], acc[:], AF.Exp, [ba, b_c0], [b_mult], bias=c0[:, h:h + 1], scale=1.0)
            else:
                ACT(P, acc[:], acc[:], AF.Exp, [ba, b_c0], [ba], bias=c0[:, h:h + 1], scale=1.0)
                TT(P, "pool", MULT[:, h, j, :], acc[:], mask[:, j - 1, :], ALU.mult, [ba, b_mask], [b_mult])
    P.pop_scope()
    Q = P.sbuf("C_Q", [128, S], BF16)
    K = P.sbuf("C_K", [128, S], BF16)
    V = P.sbuf("C_V", [128, 64, 128], BF16)
    MBT = P.sbuf("C_MBT", [32, S], BF16)
    bQ = [Buf() for _ in range(16)]
    bK = [Buf() for _ in range(16)]
    bV = [Buf() for _ in range(16)]
    bMB = [Buf() for _ in range(16)]
    kmf = P.sbuf("C_kmf", [128, 32], F32)
    kmT = P.sbuf("C_kmT", [128, 32], BF16)
    b_kmf, b_kmT = Buf(), Buf()
    gs = Slots(P, "C_g", 2, [128, 32], F32)
    mxs = Slots(P, "C_mx", 2, [128, 8], F32)
    mbs = Slots(P, "C_mb", 2, [128, 32], F32)
    psM = Slots(P, "C_psM", 2, [128, 512], F32, psum=True)
    psS = Slots(P, "C_psS", 3, [128, 512], F32, psum=True)
    psO = Slots(P, "C_psO", 2, [128, 512], F32, psum=True)
    psSUM = Slots(P, "C_psSUM", 1, [128, 512], F32, psum=True)
    pts = Slots(P, "C_pt", 5, [128, 512], BF16)
    accs = Slots(P, "C_racc", 2, [128, 512], F32)
    hls = Slots(P, "C_hl", 2, [128, 512], BF16)
    rss = Slots(P, "C_rs", 2, [128, 512], F32)
    ots = Slots(P, "C_ot", 2, [128, 512], BF16)
    VTv = VT.rearrange("(t p) d -> p t d", p=128)
    scale = float(128 ** -0.5)
    for hh in range(2):
        for tt in range(16):
            cols = slice(tt * 512, (tt + 1) * 512)
            DMA(P, "sp", Q[:, cols], QT[hh * 128:(hh + 1) * 128, cols], [], [bQ[tt]], bQ[tt])
            DMA(P, "sp", K[:, cols], KT[hh * 128:(hh + 1) * 128, cols], [], [bK[tt]], bK[tt])
            DMA(P, "sp", V[:, tt * 4:(tt + 1) * 4, :], VTv[:, tt * 4:(tt + 1) * 4, hh * 128:(hh + 1) * 128], [], [bV[tt]], bV[tt])
        P.op("dve", lambda e: e.tensor_reduce(out=kmf[:], in_=K[:].rearrange("p (n l) -> p n l", l=256), axis=AX.X, op=ALU.add),
             bK, [b_kmf])
        TS(P, "dve", kmT[:], kmf[:], float(1.0 / 256), None, ALU.mult, None, [b_kmf], [b_kmT])
        for i in range(64):
            own = i // 2
            G, bG = psM.next()
            MM(P, G[:, 0:32], Q[:, i * 128:(i + 1) * 128], kmT[:], True, True, [bQ[i // 4], b_kmT], [bG])
            g, bg = gs.next()
            CP(P, "dve", g[:], G[:, 0:32], [bG], [bg])
            MEMSET(P, "dve", g[:, own:32], -1e30, [bg])
            mx, bmx = mxs.next()
            P.op("dve", lambda e, mx=mx, g=g: e.max(out=mx[:], in_=g[:]), [bg], [bmx])
            mb, bmb = mbs.next()
            TS(P, "dve", mb[:], g[:], mx[:, 2:3], -30000.0, ALU.is_lt, ALU.mult, [bg, bmx], [bmb])
            MEMSET(P, "dve", mb[:, own:own + 1], 0.0, [bmb])
            if own + 1 < 32:
                MEMSET(P, "dve", mb[:, own + 1:32], -30000.0, [bmb])
            T, bT = psM.next()
            P.op("pe", lambda e, T=T, mb=mb: e.transpose(T[0:32, 0:128], mb[:], ident[:]), [bmb, b_ident], [bT])
            CP(P, "act", MBT[:, i * 128:(i + 1) * 128], T[0:32, 0:128], [bT], [bMB[i // 4]])

        def extra_qk(Sp, bS, qt, kt):
            n = kt // 2
            MM(P, Sp[:], eall[:, n * 128:(n + 1) * 128], MBT[:, qt * 512:(qt + 1) * 512], False, True,
               [b_eall, bMB[qt]], [bS])

        def special(qt, kt, hh=hh):
            j = kt - 4 * qt
            if j >= -1:
                return MULT[:, hh, j + 1, :], [b_mult]
            return None

        attn_loop(P, hh, scale, K, bK, Q, bQ, V, bV, OTh, ones, b_ones, psS, psO, psSUM, pts, rss, ots,
                  extra_qk, special, accs, hls)


def build_C():
    nc = new_nc()
    QT = din(nc, "QT", [256, S], BF16)
    KT = din(nc, "KT", [256, S], BF16)
    VT = din(nc, "VT", [S, 256], BF16)
    RB = din(nc, "RB", [128, 2, 32], F32)
    POSQ = din(nc, "POSQ", [128, 512], I32)
    POSK = din(nc, "POSK", [128, 5], I32)
    MASK = din(nc, "MASK", [128, 4, 512], BF16)
    EALL = din(nc, "EALL", [32, 32 * 128], BF16)
    IDENT = din(nc, "IDENT", [128, 128], F32)
    OTh = dout(nc, "OTh", [256, S], BF16)
    P = Prog(nc)
    ones, b_ones = setup_common(P)
    emit_C(P, QT, KT, VT, RB, POSQ, POSK, MASK, EALL, IDENT, OTh, ones, b_ones)
    P.finish()
    return nc


def run_moba_layer(HTs, positions, g, w_q, w_o, rel_bias, KTf, VTf, ffn):
    QTs = run_A(HTs, g[0], np.asarray(w_q, np.float32), NPBF, BF16)
    QTf = np.concatenate(QTs, axis=1)
    ncC = get_prog("C", build_C)
    pos = np.asarray(positions, np.int32).reshape(S)
    POSQ = c_(np.broadcast_to(pos[512:1024].reshape(1, 512), (128, 512)))
    POSK = c_(pos[384:1024].reshape(5, 128).T)
    MASK = causal_masks()
    EALL = c_(np.repeat(np.eye(32, dtype=np.float32)[:, :, None], 128, axis=2).reshape(32, 32 * 128).astype(NPBF))
    IDENT = np.eye(128, dtype=np.float32)
    rel_bias = np.asarray(rel_bias, np.float32)
    maps = []
    for r in range(NCORES):
        RB = c_(np.broadcast_to(rel_bias[:, 2 * r:2 * r + 2].T.reshape(1, 2, 32), (128, 2, 32)))
        maps.append({"QT": c_(QTf[r * 256:(r + 1) * 256]), "KT": c_(KTf[r * 256:(r + 1) * 256]),
                     "VT": c_(VTf[r * 256:(r + 1) * 256].T), "RB": RB, "POSQ": POSQ, "POSK": POSK,
                     "MASK": MASK, "EALL": EALL, "IDENT": IDENT})
    outs = launch(ncC, maps)
    OT_full = np.concatenate([o["OTh"] for o in outs], axis=0)
    DBG["OTm"] = OT_full
    HMs, XN2s = run_D(OT_full, HTs, w_o, g[1], g[2])
    return run_E(XN2s, HMs, *ffn, g[3])


def kernel(x, positions, norm_gains, a_w_in, a_q_norm, a_w_q_up, a_kv_norm, a_w_kv_up, a_w_o,
           b_kv_norm, b_w_kv, b_w_q, b_w_o, rel_bias, ffn_w_in, ffn_conv_w, ffn_conv_b, ffn_w_out):
    x = np.asarray(x, np.float32)
    HTs = [c_(x[0, r * TOK:(r + 1) * TOK, :].T) for r in range(NCORES)]
    KTf = VTf = None
    for layer in range(4):
        g = np.asarray(norm_gains[layer], np.float32)
        ffn = (np.asarray(ffn_w_in[layer], np.float32), ffn_conv_w[layer], ffn_conv_b[layer],
               np.asarray(ffn_w_out[layer], np.float32))
        if layer < 2:
            HTs = run_mla_layer(HTs, positions, g, a_w_in[layer], a_q_norm[layer], np.asarray(a_w_q_up[layer], np.float32),
                                a_kv_norm[layer], np.asarray(a_w_kv_up[layer], np.float32),
                                np.asarray(a_w_o[layer], np.float32), ffn)
        else:
            if layer == 2:
                KVs = run_A(HTs, b_kv_norm, np.asarray(b_w_kv, np.float32), NPBF, BF16)
                KV = np.concatenate(KVs, axis=1)
                KTf, VTf = KV[:2048], KV[2048:]
            j = layer - 2
            HTs = run_moba_layer(HTs, positions, g, b_w_q[j], np.asarray(b_w_o[j], np.float32), rel_bias, KTf, VTf, ffn)
    out = np.concatenate([h.T for h in HTs], axis=0).reshape(1, S, D).astype(np.float32)
    return out
```

```python
import math
import numpy as np
import ml_dtypes
from contextlib import ExitStack
import concourse.bass as bass
import concourse.mybir as mybir
from concourse.bass_utils import run_bass_kernel_spmd

F32 = mybir.dt.float32
BF16 = mybir.dt.bfloat16
I32 = mybir.dt.int32
ALU = mybir.AluOpType
AF = mybir.ActivationFunctionType
AX = mybir.AxisListType
NPBF = ml_dtypes.bfloat16

NCORES = 8
S = 8192
D = 2048
TOK = 1024
EPS = 1e-6
DFF = 5632
ENGS = ("pe", "act", "dve", "pool", "sp")


class Buf:
    __slots__ = ("name", "w", "r", "dsem")

    def __init__(self, name=""):
        self.name = name
        self.w = {}
        self.r = {}
        self.dsem = None


class Prog:
    def __init__(self, nc):
        self.nc = nc
        self.es = ExitStack()
        self.semstack = ExitStack()
        self.q = {e: [] for e in ENGS}
        self.sems = {}
        self.cnt = {}
        self.seen = {e: {} for e in ENGS}
        for e in ENGS:
            if e != "sp":
                self._newsem("E_" + e)
        self.nd = 0
        self.ninstr = 0

    def _newsem(self, key):
        self.sems[key] = self.semstack.enter_context(self.nc.semaphore(key))
        self.cnt[key] = 0
        return key

    def sbuf(self, name, shape, dtype):
        return self.es.enter_context(self.nc.sbuf_tensor(name, list(shape), dtype))

    def push_scope(self):
        self._outer = self.es
        self.es = ExitStack()

    def pop_scope(self):
        self.barrier()
        self.es.close()
        self.es = self._outer

    def psum(self, name, shape, dtype=F32):
        return self.es.enter_context(self.nc.psum_tensor(name, list(shape), dtype))

    def _deps(self, reads, writes):
        deps = {}
        for b in reads:
            for k, v in b.w.items():
                if deps.get(k, 0) < v:
                    deps[k] = v
        for b in writes:
            for k, v in b.w.items():
                if deps.get(k, 0) < v:
                    deps[k] = v
            for k, v in b.r.items():
                if deps.get(k, 0) < v:
                    deps[k] = v
        return deps

    def _waits(self, eng, deps):
        seen = self.seen[eng]
        own = "E_" + eng
        for k, v in deps.items():
            if k == own and eng == "pe":
                continue
            if seen.get(k, 0) < v:
                seen[k] = v
                self.q[eng].append(("wait", k, v))

    def _mark(self, key, v, reads, writes):
        for b in writes:
            b.w[key] = v
            b.r = {}
        for b in reads:
            if b.r.get(key, 0) < v:
                b.r[key] = v
        self.ninstr += 1

    def op(self, eng, fn, reads=(), writes=()):
        self._waits(eng, self._deps(reads, writes))
        key = "E_" + eng
        self.cnt[key] += 1
        self.q[eng].append(("op", fn, key, 1))
        self._mark(key, self.cnt[key], reads, writes)

    def dma(self, eng, fn, reads, writes, owner):
        self._waits(eng, self._deps(reads, writes))
        if owner.dsem is None:
            self.nd += 1
            owner.dsem = self._newsem("D%d" % self.nd)
        key = owner.dsem
        self.cnt[key] += 16
        self.q[eng].append(("op", fn, key, 16))
        self._mark(key, self.cnt[key], reads, writes)

    def barrier(self):
        for e in ENGS:
            seen = self.seen[e]
            for k, v in self.cnt.items():
                if v > 0 and seen.get(k, 0) < v:
                    seen[k] = v
                    self.q[e].append(("wait", k, v))

    def finish(self):
        self.barrier()
        engmap = {"pe": "tensor", "act": "scalar", "dve": "vector", "pool": "gpsimd", "sp": "sync"}
        with self.nc.Block() as block:
            for e in ENGS:
                def body(engine, items=self.q[e]):
                    for it in items:
                        if it[0] == "wait":
                            engine.wait_ge(self.sems[it[1]], it[2])
                        else:
                            it[1](engine).then_inc(self.sems[it[2]], it[3])
                getattr(block, engmap[e])(body)
        self.es.close()
        self.semstack.close()


class Slots:
    def __init__(self, P, name, n, shape, dtype, psum=False):
        self.items = []
        for i in range(n):
            t = P.psum(f"{name}{i}", shape, dtype) if psum else P.sbuf(f"{name}{i}", shape, dtype)
            self.items.append((t, Buf(f"{name}{i}")))
        self.i = 0

    def next(self):
        it = self.items[self.i % len(self.items)]
        self.i += 1
        return it


def MM(P, out, lhsT, rhs, start, stop, r, w):
    P.op("pe", lambda e: e.matmul(out, lhsT, rhs, start=start, stop=stop), r, w)


def ACT(P, out, in_, func, r, w, bias=None, scale=None):
    kw = {}
    if bias is not None:
        kw["bias"] = bias
    if scale is not None:
        kw["scale"] = scale
    P.op("act", lambda e: e.activation(out=out, in_=in_, func=func, **kw), r, w)


def TT(P, eng, out, in0, in1, op, r, w):
    P.op(eng, lambda e: e.tensor_tensor(out=out, in0=in0, in1=in1, op=op), r, w)


def TS(P, eng, out, in0, s1, s2, op0, op1, r, w):
    if s2 is None:
        P.op(eng, lambda e: e.tensor_scalar(out=out, in0=in0, scalar1=s1, scalar2=None, op0=op0), r, w)
    else:
        P.op(eng, lambda e: e.tensor_scalar(out=out, in0=in0, scalar1=s1, scalar2=s2, op0=op0, op1=op1), r, w)


def STT(P, eng, out, in0, scalar, in1, op0, op1, r, w):
    eng = "dve"
    P.op(eng, lambda e: e.scalar_tensor_tensor(out=out, in0=in0, scalar=scalar, in1=in1, op0=op0, op1=op1), r, w)


def CP(P, eng, out, in_, r, w):
    if eng == "act":
        P.op("act", lambda e: e.activation(out=out, in_=in_, func=AF.Identity), r, w)
    else:
        P.op(eng, lambda e: e.tensor_copy(out=out, in_=in_), r, w)


def MEMSET(P, eng, out, val, w):
    P.op(eng, lambda e: e.memset(out, val), [], w)


def DMA(P, q, out, in_, r, w, owner):
    P.dma(q, lambda e: e.dma_start(out=out, in_=in_), r, w, owner)


def RSTD(P, ssq_ap, n, dim, tmp, b_ssq, b_tmp):
    ACT(P, tmp, ssq_ap, AF.Sqrt, [b_ssq], [b_tmp], bias=EPS, scale=1.0 / dim)
    P.op("dve", lambda e: e.reciprocal(out=tmp, in_=tmp), [b_tmp], [b_tmp])


def new_nc():
    return bass.Bass("TRN2", target_bir_lowering=False)


def din(nc, name, shape, dt):
    return nc.dram_tensor(name, list(shape), dt, kind="ExternalInput").ap()


def dout(nc, name, shape, dt):
    return nc.dram_tensor(name, list(shape), dt, kind="ExternalOutput").ap()


def dscratch(nc, name, shape, dt):
    return nc.dram_tensor(name, list(shape), dt, kind="Internal").ap()


def setup_common(P):
    ones = P.sbuf("ones", [128, 128], BF16)
    b_ones = Buf("ones")
    MEMSET(P, "pool", ones[:], 1.0, [b_ones])
    return ones, b_ones


def emit_A(P, HT, G, W, OT, ncols, out_dt, ones, b_ones):
    g_sb = P.sbuf("A_g", [128, 16], F32)
    b_g = Buf()
    DMA(P, "sp", g_sb[:], G, [], [b_g], b_g)
    xn = P.sbuf("A_xn", [128, 16, TOK], BF16)
    b_xn = [[Buf() for _ in range(16)] for _ in range(2)]
    hst = Slots(P, "A_h", 2, [128, 16, 512], F32)
    sqs = Slots(P, "A_sq", 2, [128, 512], BF16)
    rts = Slots(P, "A_rt", 2, [128, 512], F32)
    psq = Slots(P, "A_psq", 1, [128, 512], F32, psum=True)
    pso = Slots(P, "A_pso", 4, [128, 512], F32, psum=True)
    HTv = HT.rearrange("(c p) t -> p c t", p=128)
    for p in range(2):
        h, bh = hst.next()
        DMA(P, "sp", h[:], HTv[:, :, p * 512:(p + 1) * 512], [], [bh], bh)
        ssq, bq = psq.next()
        for c in range(16):
            s, bs = sqs.next()
            ACT(P, s[:], h[:, c, :], AF.Square, [bh], [bs])
            MM(P, ssq[:], ones[:], s[:], c == 0, c == 15, [b_ones, bs], [bq])
        rt, brt = rts.next()
        RSTD(P, ssq[:], 512, D, rt[:], bq, brt)
        for c in range(16):
            STT(P, "dve" if c % 2 == 0 else "pool", xn[:, c, p * 512:(p + 1) * 512], h[:, c, :], g_sb[:, c:c + 1], rt[:],
                ALU.mult, ALU.mult, [bh, b_g, brt], [b_xn[p][c]])
    Wv = W.rearrange("(kc p) n -> p kc n", p=128)
    wsl = Slots(P, "A_w", 3, [128, 16, 512], BF16)
    ost = Slots(P, "A_o", 4, [128, 512], out_dt)
    k = 0
    for c0 in range(0, ncols, 512):
        wc = min(512, ncols - c0)
        wt, bw = wsl.next()
        DMA(P, "pool", wt[:, :, 0:wc], Wv[:, :, c0:c0 + wc], [], [bw], bw)
        for p in range(2):
            for j in range(wc // 128):
                ps, bp = pso.next()
                for kc in range(16):
                    MM(P, ps[:], wt[:, kc, j * 128:(j + 1) * 128], xn[:, kc, p * 512:(p + 1) * 512], kc == 0, kc == 15,
                       [bw, b_xn[p][kc]], [bp])
                o, bo = ost.next()
                CP(P, "act" if k % 2 == 0 else "dve", o[:], ps[:], [bp], [bo])
                k += 1
                r0 = c0 + j * 128
                DMA(P, "sp", OT[r0:r0 + 128, p * 512:(p + 1) * 512], o[:], [bo], [], bo)


def build_A(ncols, out_dt):
    nc = new_nc()
    HT = din(nc, "HT", [D, TOK], F32)
    G = din(nc, "G", [128, 16], F32)
    W = din(nc, "W", [D, ncols], F32)
    OT = dout(nc, "OT", [ncols, TOK], out_dt)
    P = Prog(nc)
    ones, b_ones = setup_common(P)
    emit_A(P, HT, G, W, OT, ncols, out_dt, ones, b_ones)
    P.finish()
    return nc


def emit_D(P, OTin, HT, WO, G1, G2, HM, XN2, ones, b_ones):
    g1 = P.sbuf("D_g1", [128, 16], F32)
    g2 = P.sbuf("D_g2", [128, 16], F32)
    b_g1, b_g2 = Buf(), Buf()
    DMA(P, "sp", g1[:], G1, [], [b_g1], b_g1)
    DMA(P, "sp", g2[:], G2, [], [b_g2], b_g2)
    o = P.sbuf("D_o", [128, 16, TOK], BF16)
    b_o = Buf()
    OTv = OTin.rearrange("(c p) t -> p c t", p=128)
    for c in range(16):
        DMA(P, "sp", o[:, c, :], OTv[:, c, :], [], [b_o], b_o)
    mix = P.sbuf("D_mix", [128, 16, TOK], F32)
    b_mix = [[Buf() for _ in range(16)] for _ in range(2)]
    wsl = Slots(P, "D_w", 3, [128, 16, 512], BF16)
    pso = Slots(P, "D_pso", 3, [128, 512], F32, psum=True)
    pss = [P.psum(f"D_pss{i}", [128, 512], F32) for i in range(2)]
    b_pss = [Buf(), Buf()]
    pss2 = [P.psum(f"D_pss2{i}", [128, 512], F32) for i in range(2)]
    b_pss2 = [Buf(), Buf()]
    sqs = Slots(P, "D_sq", 3, [128, 512], BF16)
    WOv = WO.rearrange("(kc p) n -> p kc n", p=128)
    for wg in range(4):
        wt, bw = wsl.next()
        DMA(P, "pool", wt[:], WOv[:, :, wg * 512:(wg + 1) * 512], [], [bw], bw)
        for p in range(2):
            for j in range(4):
                oc = wg * 4 + j
                ps, bp = pso.next()
                for kc in range(16):
                    MM(P, ps[:], wt[:, kc, j * 128:(j + 1) * 128], o[:, kc, p * 512:(p + 1) * 512], kc == 0, kc == 15,
                       [bw, b_o], [bp])
                CP(P, "dve", mix[:, oc, p * 512:(p + 1) * 512], ps[:], [bp], [b_mix[p][oc]])
                s, bs = sqs.next()
                ACT(P, s[:], mix[:, oc, p * 512:(p + 1) * 512], AF.Square, [b_mix[p][oc]], [bs])
                MM(P, pss[p][:], ones[:], s[:], oc == 0, oc == 15, [b_ones, bs], [b_pss[p]])
    import os
    DSTOP = int(os.environ.get("D_STOP", "9"))
    if DSTOP <= 1:
        return
    rt1 = P.sbuf("D_rt1", [128, TOK], F32)
    b_rt1 = [Buf(), Buf()]
    for p in range(2):
        RSTD(P, pss[p][:], 512, D, rt1[:, p * 512:(p + 1) * 512], b_pss[p], b_rt1[p])
    hst = Slots(P, "D_h", 3, [128, TOK], F32)
    HTv = HT.rearrange("(c p) t -> p c t", p=128)
    HMv = HM.rearrange("(c p) t -> p c t", p=128)
    for c in range(16):
        h, bh = hst.next()
        DMA(P, "sp", h[:], HTv[:, c, :], [], [bh], bh)
        for p in range(2):
            sl = slice(p * 512, (p + 1) * 512)
            STT(P, "dve", mix[:, c, sl], mix[:, c, sl], g1[:, c:c + 1], rt1[:, sl], ALU.mult, ALU.mult,
                [b_mix[p][c], b_g1, b_rt1[p]], [b_mix[p][c]])
            TT(P, "dve", mix[:, c, sl], mix[:, c, sl], h[:, sl], ALU.add, [b_mix[p][c], bh], [b_mix[p][c]])
            s, bs = sqs.next()
            ACT(P, s[:], mix[:, c, sl], AF.Square, [b_mix[p][c]], [bs])
            MM(P, pss2[p][:], ones[:], s[:], c == 0, c == 15, [b_ones, bs], [b_pss2[p]])
        DMA(P, "sp", HMv[:, c, :], mix[:, c, :], [b_mix[0][c], b_mix[1][c]], [], bh)
    if DSTOP <= 2:
        return
    rt2 = P.sbuf("D_rt2", [128, TOK], F32)
    b_rt2 = [Buf(), Buf()]
    for p in range(2):
        RSTD(P, pss2[p][:], 512, D, rt2[:, p * 512:(p + 1) * 512], b_pss2[p], b_rt2[p])
    xst = Slots(P, "D_x", 3, [128, TOK], BF16)
    XNv = XN2.rearrange("(c p) t -> p c t", p=128)
    for c in range(16):
        x, bx = xst.next()
        for p in range(2):
            sl = slice(p * 512, (p + 1) * 512)
            STT(P, "dve" if p == 0 else "pool", x[:, sl], mix[:, c, sl], g2[:, c:c + 1], rt2[:, sl], ALU.mult, ALU.mult,
                [b_mix[p][c], b_g2, b_rt2[p]], [bx])
        DMA(P, "sp", XNv[:, c, :], x[:], [bx], [], bx)


def build_D():
    nc = new_nc()
    OTin = din(nc, "OTin", [D, TOK], BF16)
    HT = din(nc, "HT", [D, TOK], F32)
    WO = din(nc, "WO", [D, D], F32)
    G1 = din(nc, "G1", [128, 16], F32)
    G2 = din(nc, "G2", [128, 16], F32)
    HM = dout(nc, "HM", [D, TOK], F32)
    XN2 = dout(nc, "XN2", [D, TOK], BF16)
    P = Prog(nc)
    ones, b_ones = setup_common(P)
    emit_D(P, OTin, HT, WO, G1, G2, HM, XN2, ones, b_ones)
    P.finish()
    return nc


def emit_E(P, XN2H, HM, WIN, CW, CB, WOUT, G3, FT, HOUT, ones, b_ones):
    g3 = P.sbuf("E_g3", [128, 16], F32)
    b_g3 = Buf()
    DMA(P, "sp", g3[:], G3, [], [b_g3], b_g3)
    cw = P.sbuf("E_cw", [128, 88, 3], F32)
    cb = P.sbuf("E_cb", [128, 88], F32)
    b_cw, b_cb = Buf(), Buf()
    DMA(P, "sp", cw[:], CW, [], [b_cw], b_cw)
    DMA(P, "sp", cb[:], CB, [], [b_cb], b_cb)
    NT = TOK + 2
    xn = P.sbuf("E_xn", [128, 16, NT], BF16)
    b_xn = Buf()
    DMA(P, "sp", xn[:], XN2H.rearrange("(c p) t -> p c t", p=128), [], [b_xn], b_xn)
    act = P.sbuf("E_act", [128, 44, TOK], BF16)
    b_act = [Buf() for _ in range(44)]
    wsl = Slots(P, "E_w", 6, [128, 4096], BF16)
    psA = Slots(P, "E_psA", 5, [128, 512], F32, psum=True)
    psC = Slots(P, "E_psC", 1, [128, 512], F32, psum=True)
    pss = [P.psum(f"E_pss{i}", [128, 512], F32) for i in range(2)]
    b_pss = [Buf(), Buf()]
    hss = Slots(P, "E_hs", 2, [128, NT], F32)
    tss = Slots(P, "E_t", 2, [128, TOK], F32)
    gls = Slots(P, "E_gl", 2, [128, TOK], F32)
    WINv = WIN.rearrange("(kc p) n -> p kc n", p=128)
    WOv = WOUT.rearrange("(kc p) n -> p kc n", p=128)
    tiles = []
    for g2 in range(22):
        tiles.append(("in", g2 * 256))
        tiles.append(("in", DFF + g2 * 256))
    for dc in range(16):
        tiles.append(("out", dc, 0))
        tiles.append(("out", dc, 1))
    loaded = {}

    def issue(i):
        if i >= len(tiles):
            return
        w, bw = wsl.next()
        td = tiles[i]
        if td[0] == "in":
            v = w[:].rearrange("p (k n) -> p k n", n=256)
            DMA(P, "pool", v, WINv[:, :, td[1]:td[1] + 256], [], [bw], bw)
        else:
            v = w[:, 0:22 * 128].rearrange("p (k n) -> p k n", n=128)
            DMA(P, "pool", v, WOv[:, td[2] * 22:(td[2] + 1) * 22, td[1] * 128:(td[1] + 1) * 128], [], [bw], bw)
        loaded[i] = (v, bw)

    for i in range(6):
        issue(i)
    for g2 in range(22):
        wgv, bwg = loaded[2 * g2]
        wuv, bwu = loaded[2 * g2 + 1]
        for j in range(2):
            gl_keep = None
            for which in range(2):
                wv, bw = (wgv, bwg) if which == 0 else (wuv, bwu)
                fidx = (g2 * 2 + j) + (0 if which == 0 else 44)
                hs, bhs = hss.next()
                pa, bpa = psA.next()
                pb, bpb = psA.next()
                pc, bpc = psC.next()
                for (ps, bp, c0, n) in ((pa, bpa, 0, 512), (pb, bpb, 512, 512), (pc, bpc, 1024, 2)):
                    for kc in range(16):
                        MM(P, ps[:, 0:n], wv[:, kc, j * 128:(j + 1) * 128], xn[:, kc, c0:c0 + n], kc == 0, kc == 15,
                           [bw, b_xn], [bp])
                CP(P, "act", hs[:, 0:512], pa[:], [bpa], [bhs])
                CP(P, "act", hs[:, 512:1024], pb[:], [bpb], [bhs])
                CP(P, "act", hs[:, 1024:1026], pc[:, 0:2], [bpc], [bhs])
                t, bt = tss.next()
                TS(P, "dve", t[:], hs[:, 0:TOK], cw[:, fidx, 0:1], cb[:, fidx:fidx + 1], ALU.mult, ALU.add,
                   [bhs, b_cw, b_cb], [bt])
                STT(P, "dve", t[:], hs[:, 1:TOK + 1], cw[:, fidx, 1:2], t[:], ALU.mult, ALU.add, [bhs, b_cw, bt], [bt])
                STT(P, "dve", t[:], hs[:, 2:TOK + 2], cw[:, fidx, 2:3], t[:], ALU.mult, ALU.add, [bhs, b_cw, bt], [bt])
                if which == 0:
                    gl, bgl = gls.next()
                    ACT(P, gl[:], t[:], AF.Gelu_apprx_tanh, [bt], [bgl])
                    gl_keep = (gl, bgl)
                else:
                    gl, bgl = gl_keep
                    kidx = g2 * 2 + j
                    TT(P, "dve", act[:, kidx, :], gl[:], t[:], ALU.mult, [bgl, bt], [b_act[kidx]])
        issue(2 * g2 + 6)
        issue(2 * g2 + 7)
    fst = Slots(P, "E_f", 2, [128, 512], F32)
    sqs = Slots(P, "E_sq", 3, [128, 512], BF16)
    FTv = FT.rearrange("(c p) t -> p c t", p=128)
    b_ft = [Buf() for _ in range(16)]
    for dc in range(16):
        base = 44 + 2 * dc
        (w0, b0), (w1, b1) = loaded[base], loaded[base + 1]
        for p in range(2):
            ps, bp = psA.next()
            for k in range(44):
                wv, bw = (w0, b0) if k < 22 else (w1, b1)
                MM(P, ps[:], wv[:, k % 22, :], act[:, k, p * 512:(p + 1) * 512], k == 0, k == 43, [bw, b_act[k]], [bp])
            f, bf = fst.next()
            CP(P, "dve", f[:], ps[:], [bp], [bf])
            s, bs = sqs.next()
            ACT(P, s[:], f[:], AF.Square, [bf], [bs])
            MM(P, pss[p][:], ones[:], s[:], dc == 0, dc == 15, [b_ones, bs], [b_pss[p]])
            DMA(P, "sp", FTv[:, dc, p * 512:(p + 1) * 512], f[:], [bf], [b_ft[dc]], bf)
        issue(base + 6)
        issue(base + 7)
    rt3 = P.sbuf("E_rt3", [128, TOK], F32)
    b_rt3 = [Buf(), Buf()]
    for p in range(2):
        RSTD(P, pss[p][:], 512, D, rt3[:, p * 512:(p + 1) * 512], b_pss[p], b_rt3[p])
    HMv = HM.rearrange("(c p) t -> p c t", p=128)
    HOv = HOUT.rearrange("(c p) t -> p c t", p=128)
    for c in range(16):
        f, bf = tss.next()
        hm, bhm = gls.next()
        DMA(P, "sp", f[:], FTv[:, c, :], [b_ft[c]], [bf], bf)
        DMA(P, "sp", hm[:], HMv[:, c, :], [], [bhm], bhm)
        for p in range(2):
            sl = slice(p * 512, (p + 1) * 512)
            STT(P, "dve", f[:, sl], f[:, sl], g3[:, c:c + 1], rt3[:, sl], ALU.mult, ALU.mult, [bf, b_g3, b_rt3[p]], [bf])
        TT(P, "pool", f[:], f[:], hm[:], ALU.add, [bf, bhm], [bf])
        DMA(P, "sp", HOv[:, c, :], f[:], [bf], [], bf)


def build_E():
    nc = new_nc()
    XN2H = din(nc, "XN2H", [D, TOK + 2], BF16)
    HM = din(nc, "HM", [D, TOK], F32)
    WIN = din(nc, "WIN", [D, 2 * DFF], F32)
    CW = din(nc, "CW", [128, 88, 3], F32)
    CB = din(nc, "CB", [128, 88], F32)
    WOUT = din(nc, "WOUT", [DFF, D], F32)
    G3 = din(nc, "G3", [128, 16], F32)
    FT = dscratch(nc, "FT", [D, TOK], F32)
    HOUT = dout(nc, "HOUT", [D, TOK], F32)
    P = Prog(nc)
    ones, b_ones = setup_common(P)
    emit_E(P, XN2H, HM, WIN, CW, CB, WOUT, G3, FT, HOUT, ones, b_ones)
    P.finish()
    return nc


def attn_loop(P, hh, scale, KN, bKN, QN, bQN, V, bV, OTh, ones, b_ones, psS, psO, psSUM, pts, rss, ots,
              extra_qk, special):
    pairs = [(qt, kt) for qt in range(16) for kt in range(4 * qt + 4)]
    state = {}
    LOOK = 3

    def emit_pv(qt, kt, pt, bpt):
        if kt == 0:
            state["O"] = psO.next()
            state["SUM"] = psSUM.next()
        O, bO = state["O"]
        SUM, bSUM = state["SUM"]
        last = kt == 4 * qt + 3
        MM(P, O[:], V[:, kt, :], pt[:], kt == 0, last, [bV[kt // 4], bpt], [bO])
        MM(P, SUM[:], ones[:], pt[:], kt == 0, last, [b_ones, bpt], [bSUM])
        if last:
            rs, brs = rss.next()
            P.op("dve", lambda e: e.reciprocal(out=rs[:], in_=SUM[:]), [bSUM], [brs])
            ot, bot = ots.next()
            TT(P, "dve", ot[:], O[:], rs[:], ALU.mult, [bO, brs], [bot])
            DMA(P, "sp", OTh[hh * 128:(hh + 1) * 128, qt * 512:(qt + 1) * 512], ot[:], [bot], [], bot)

    ring = []
    n = len(pairs)
    for idx in range(n + LOOK):
        if idx < n:
            qt, kt = pairs[idx]
            Sp, bS = psS.next()
            MM(P, Sp[:], KN[:, kt * 128:(kt + 1) * 128], QN[:, qt * 512:(qt + 1) * 512], True, False,
               [bKN[kt // 4], bQN[qt]], [bS])
            extra_qk(Sp, bS, qt, kt)
            pt, bpt = pts.next()
            ACT(P, pt[:], Sp[:], AF.Exp, [bS], [bpt], scale=scale)
            sp = special(qt, kt)
            if sp is not None:
                m_ap, m_bufs = sp
                TT(P, "dve", pt[:], pt[:], m_ap, ALU.mult, [bpt] + m_bufs, [bpt])
            ring.append((qt, kt, pt, bpt))
        if idx >= LOOK:
            emit_pv(*ring[idx - LOOK])


C1 = 6.28125
C2 = 2 * math.pi - 6.28125


def emit_rope_tables(P, POS, INV, SGN, ROPE):
    inv = P.sbuf("R_inv", [64, 1], F32)
    sgn = P.sbuf("R_sgn", [64, 1], F32)
    b_inv, b_sgn = Buf(), Buf()
    DMA(P, "sp", inv[:], INV, [], [b_inv], b_inv)
    DMA(P, "sp", sgn[:], SGN, [], [b_sgn], b_sgn)
    pis = Slots(P, "R_pi", 2, [64, 512], I32)
    angs = Slots(P, "R_ang", 2, [64, 512], F32)
    xs = Slots(P, "R_x", 4, [64, 512], F32)
    nfs = Slots(P, "R_nf", 4, [64, 512], F32)
    nis = Slots(P, "R_ni", 4, [64, 512], I32)
    b_rope = [[Buf() for _ in range(16)] for _ in range(2)]
    for tt in range(16):
        cols = slice(tt * 512, (tt + 1) * 512)
        pi, bpi = pis.next()
        DMA(P, "sp", pi[:], POS[:, cols], [], [bpi], bpi)
        ang, bang = angs.next()
        CP(P, "dve", ang[:], pi[:], [bpi], [bang])
        TS(P, "dve", ang[:], ang[:], inv[:, 0:1], None, ALU.mult, None, [bang, b_inv], [bang])
        for which in range(2):
            eng = "dve"
            x, bx = xs.next()
            nf, bnf = nfs.next()
            ni, bni = nis.next()
            if which == 0:
                TS(P, eng, x[:], ang[:], float(math.pi / 2), None, ALU.add, None, [bang], [bx])
            else:
                CP(P, eng, x[:], ang[:], [bang], [bx])
            TS(P, eng, nf[:], x[:], float(1.0 / (2 * math.pi)), None, ALU.mult, None, [bx], [bnf])
            CP(P, eng, ni[:], nf[:], [bnf], [bni])
            CP(P, eng, nf[:], ni[:], [bni], [bnf])
            STT(P, eng, x[:], nf[:], -C1, x[:], ALU.mult, ALU.add, [bnf, bx], [bx])
            STT(P, eng, x[:], nf[:], -float(C2), x[:], ALU.mult, ALU.add, [bnf, bx], [bx])
            TS(P, eng, x[:], x[:], float(math.pi), -float(math.pi), ALU.min, ALU.max, [bx], [bx])
            ACT(P, x[:], x[:], AF.Sin, [bx], [bx])
            if which == 1:
                TS(P, eng, x[:], x[:], sgn[:, 0:1], None, ALU.mult, None, [bx, b_sgn], [bx])
            DMA(P, "sp", ROPE[which, :, cols], x[:], [bx], [b_rope[which][tt]], bx)
    return b_rope


def emit_B(P, CT, QG, KG, WQ, WKV, POS, INV, SGN, MASK, ROPE, OTh, ones, b_ones):
    P.push_scope()
    b_rope = emit_rope_tables(P, POS, INV, SGN, ROPE)
    P.pop_scope()
    qg = P.sbuf("B_qg", [128, 4], F32)
    kg = P.sbuf("B_kg", [128, 4], F32)
    b_qg, b_kg = Buf(), Buf()
    DMA(P, "sp", qg[:], QG, [], [b_qg], b_qg)
    DMA(P, "sp", kg[:], KG, [], [b_kg], b_kg)
    wq = P.sbuf("B_wq", [128, 4, 512], BF16)
    wkv = P.sbuf("B_wkv", [128, 4, 512], BF16)
    b_wq, b_wkv = Buf(), Buf()
    DMA(P, "pool", wq[:], WQ.rearrange("(kc p) n -> p kc n", p=128), [], [b_wq], b_wq)
    DMA(P, "pool", wkv[:], WKV.rearrange("(kc p) n -> p kc n", p=128), [], [b_wkv], b_wkv)
    mask = P.sbuf("B_mask", [128, 4, 512], BF16)
    b_mask = Buf()
    DMA(P, "sp", mask[:], MASK, [], [b_mask], b_mask)
    QN = P.sbuf("B_QN", [128, S], BF16)
    QR = P.sbuf("B_QR", [64, S], BF16)
    KN = P.sbuf("B_KN", [128, S], BF16)
    KR = P.sbuf("B_KR", [64, S], BF16)
    V = P.sbuf("B_V", [128, 64, 128], BF16)
    bQN = [Buf() for _ in range(16)]
    bQR = [Buf() for _ in range(16)]
    bKN = [Buf() for _ in range(16)]
    bKR = [Buf() for _ in range(16)]
    bV = [Buf() for _ in range(16)]
    cqs = Slots(P, "B_cq", 2, [128, 4, 512], F32)
    ckvs = Slots(P, "B_ckv", 2, [128, 4, 512], F32)
    krs_ = Slots(P, "B_kr", 2, [64, 2, 512], F32)
    rps = Slots(P, "B_rp", 2, [64, 2, 512], F32)
    cqn = Slots(P, "B_cqn", 2, [128, 4, 512], BF16)
    ckvn = Slots(P, "B_ckvn", 2, [128, 4, 512], BF16)
    sqs = Slots(P, "B_sq", 3, [128, 512], BF16)
    rts = Slots(P, "B_rt", 2, [128, 512], F32)
    tms = Slots(P, "B_tm", 4, [64, 512], F32)
    psS = Slots(P, "B_psS", 4, [128, 512], F32, psum=True)
    psM = psS
    psO = Slots(P, "B_psO", 2, [128, 512], F32, psum=True)
    psSUM = Slots(P, "B_psSUM", 2, [128, 512], F32, psum=True)
    pts = Slots(P, "B_pt", 6, [128, 512], BF16)
    rss = Slots(P, "B_rs", 2, [128, 512], F32)
    ots = Slots(P, "B_ot", 2, [128, 512], BF16)
    CTv = CT[0:1024, :].rearrange("(c p) t -> p c t", p=128)
    scale = float(192 ** -0.5)
    for hh in range(2):
        for tt in range(16):
            cols = slice(tt * 512, (tt + 1) * 512)
            cq, bcq = cqs.next()
            DMA(P, "sp", cq[:], CTv[:, 0:4, cols], [], [bcq], bcq)
            ckv, bckv = ckvs.next()
            DMA(P, "sp", ckv[:], CTv[:, 4:8, cols], [], [bckv], bckv)
            kr, bkr = krs_.next()
            DMA(P, "sp", kr[:], CT[1024:1152, cols].rearrange("(j p) t -> p j t", p=64), [], [bkr], bkr)
            rp, brp = rps.next()
            DMA(P, "sp", rp[:], ROPE[:, :, cols].rearrange("w p t -> p w t"), [b_rope[0][tt], b_rope[1][tt]], [brp], brp)
            outs = []
            for (src, bsrc, gsb, bg, dsts) in ((cq, bcq, qg, b_qg, cqn), (ckv, bckv, kg, b_kg, ckvn)):
                ssq, bq = psM.next()
                for c in range(4):
                    s, bs = sqs.next()
                    ACT(P, s[:], src[:, c, :], AF.Square, [bsrc], [bs])
                    MM(P, ssq[:], ones[:], s[:], c == 0, c == 3, [b_ones, bs], [bq])
                rt, brt = rts.next()
                RSTD(P, ssq[:], 512, 512, rt[:], bq, brt)
                dn, bdn = dsts.next()
                for c in range(4):
                    STT(P, "dve" if c % 2 == 0 else "pool", dn[:, c, :], src[:, c, :], gsb[:, c:c + 1], rt[:],
                        ALU.mult, ALU.mult, [bsrc, bg, brt], [bdn])
                outs.append((dn, bdn))
            (qn_, bqn_), (kvn_, bkvn_) = outs
            h0 = hh * 256
            ps, bp = psM.next()
            for kc in range(4):
                MM(P, ps[:], wq[:, kc, h0:h0 + 128], qn_[:, kc, :], kc == 0, kc == 3, [b_wq, bqn_], [bp])
            CP(P, "act", QN[:, cols], ps[:], [bp], [bQN[tt]])
            pa, bpa = psM.next()
            for kc in range(4):
                MM(P, pa[0:64, :], wq[:, kc, h0 + 128:h0 + 192], qn_[:, kc, :], kc == 0, kc == 3, [b_wq, bqn_], [bpa])
            t1, bt1 = tms.next()
            TT(P, "dve", t1[:], pa[0:64, :], rp[:, 0, :], ALU.mult, [bpa, brp], [bt1])
            pb, bpb = psM.next()
            for kc in range(4):
                MM(P, pb[0:64, :], wq[:, kc, h0 + 192:h0 + 256], qn_[:, kc, :], kc == 0, kc == 3, [b_wq, bqn_], [bpb])
            t2, bt2 = tms.next()
            TT(P, "dve", t2[:], pb[0:64, :], rp[:, 1, :], ALU.mult, [bpb, brp], [bt2])
            TT(P, "pool", QR[:, cols], t1[:], t2[:], ALU.add, [bt1, bt2], [bQR[tt]])
            ps, bp = psM.next()
            for kc in range(4):
                MM(P, ps[:], wkv[:, kc, h0:h0 + 128], kvn_[:, kc, :], kc == 0, kc == 3, [b_wkv, bkvn_], [bp])
            CP(P, "act", KN[:, cols], ps[:], [bp], [bKN[tt]])
            ps, bp = psM.next()
            for k4 in range(4):
                for kc in range(4):
                    MM(P, ps[:, k4 * 128:(k4 + 1) * 128], kvn_[:, kc, k4 * 128:(k4 + 1) * 128], wkv[:, kc, h0 + 128:h0 + 256],
                       kc == 0, kc == 3, [b_wkv, bkvn_], [bp])
            CP(P, "dve", V[:, tt * 4:(tt + 1) * 4, :], ps[:].rearrange("p (a b) -> p a b", b=128), [bp], [bV[tt]])
            t1, bt1 = tms.next()
            TT(P, "pool", t1[:], kr[:, 0, :], rp[:, 0, :], ALU.mult, [bkr, brp], [bt1])
            t2, bt2 = tms.next()
            TT(P, "pool", t2[:], kr[:, 1, :], rp[:, 1, :], ALU.mult, [bkr, brp], [bt2])
            TT(P, "pool", KR[:, cols], t1[:], t2[:], ALU.add, [bt1, bt2], [bKR[tt]])

        def extra_qk(Sp, bS, qt, kt):
            MM(P, Sp[:], KR[:, kt * 128:(kt + 1) * 128], QR[:, qt * 512:(qt + 1) * 512], False, True,
               [bKR[kt // 4], bQR[qt]], [bS])

        def special(qt, kt):
            j = kt - 4 * qt
            if j >= 0:
                return mask[:, j, :], [b_mask]
            return None

        attn_loop(P, hh, scale, KN, bKN, QN, bQN, V, bV, OTh, ones, b_ones, psS, psO, psSUM, pts, rss, ots,
                  extra_qk, special)


def build_B():
    nc = new_nc()
    CT = din(nc, "CT", [1152, S], F32)
    QG = din(nc, "QG", [128, 4], F32)
    KG = din(nc, "KG", [128, 4], F32)
    WQ = din(nc, "WQ", [512, 512], F32)
    WKV = din(nc, "WKV", [512, 512], F32)
    POS = din(nc, "POS", [64, S], I32)
    INV = din(nc, "INV", [64, 1], F32)
    SGN = din(nc, "SGN", [64, 1], F32)
    MASK = din(nc, "MASK", [128, 4, 512], BF16)
    ROPE = dscratch(nc, "ROPE", [2, 64, S], F32)
    OTh = dout(nc, "OTh", [256, S], BF16)
    P = Prog(nc)
    ones, b_ones = setup_common(P)
    emit_B(P, CT, QG, KG, WQ, WKV, POS, INV, SGN, MASK, ROPE, OTh, ones, b_ones)
    P.finish()
    return nc


_PROGS = {}


def get_prog(key, builder):
    if key not in _PROGS:
        _PROGS[key] = builder()
    return _PROGS[key]


def launch(nc, in_maps):
    res = run_bass_kernel_spmd(nc, in_maps, core_ids=list(range(NCORES)))
    return res.results


def gchunk(g):
    return np.ascontiguousarray(np.asarray(g, np.float32).reshape(-1, 128).T)


def causal_masks():
    kp = np.arange(128)[:, None, None]
    j = np.arange(4)[None, :, None]
    qf = np.arange(512)[None, None, :]
    return (128 * j + kp <= qf).astype(np.float32).astype(NPBF)


def c_(a):
    return np.ascontiguousarray(a)


def run_A(HTs, g, W, out_np_dt, out_dt):
    ncols = W.shape[1]
    nc = get_prog(("A", ncols, str(out_dt)), lambda: build_A(ncols, out_dt))
    G = gchunk(g)
    W = c_(W.astype(np.float32))
    outs = launch(nc, [{"HT": HTs[r], "G": G, "W": W} for r in range(NCORES)])
    return [o["OT"] for o in outs]


def run_D(OT_full, HTs, WO, g1, g2):
    nc = get_prog("D", build_D)
    WO = c_(WO)
    G1, G2 = gchunk(g1), gchunk(g2)
    outs = launch(nc, [{"OTin": c_(OT_full[:, r * TOK:(r + 1) * TOK]), "HT": HTs[r], "WO": WO, "G1": G1, "G2": G2}
                       for r in range(NCORES)])
    return [o["HM"] for o in outs], [o["XN2"] for o in outs]


def run_E(XN2s, HMs, WIN, CWc, CBc, WOUT, g3):
    nc = get_prog("E", build_E)
    CW = c_(np.asarray(CWc, np.float32).T.reshape(88, 128, 3).transpose(1, 0, 2))
    CB = c_(np.asarray(CBc, np.float32).reshape(88, 128).T)
    G3 = gchunk(g3)
    WIN, WOUT = c_(WIN), c_(WOUT)
    maps = []
    for r in range(NCORES):
        halo = np.zeros((D, 2), NPBF) if r == 0 else XN2s[r - 1][:, TOK - 2:TOK]
        maps.append({"XN2H": c_(np.concatenate([halo, XN2s[r]], axis=1)), "HM": HMs[r], "WIN": WIN, "CW": CW,
                     "CB": CB, "WOUT": WOUT, "G3": G3})
    outs = launch(nc, maps)
    return [o["HOUT"] for o in outs]


ROPE_PERM = np.concatenate([np.arange(32, 64), np.arange(0, 32)])


DBG = {}


def run_mla_layer(HTs, positions, g, w_in, q_norm, w_q_up, kv_norm, w_kv_up, w_o, ffn):
    w_in = np.asarray(w_in, np.float32)
    Wext = np.concatenate([w_in, w_in[:, 1024 + ROPE_PERM]], axis=1)
    CTs = run_A(HTs, g[0], Wext, np.float32, F32)
    CT = c_(np.concatenate(CTs, axis=1))
    DBG["CT"] = CT
    ncB = get_prog("B", build_B)
    POS = c_(np.broadcast_to(np.asarray(positions, np.int32).reshape(1, S), (64, S)))
    inv = (np.float32(10000.0) ** (-np.arange(32, dtype=np.float32) / np.float32(32))).astype(np.float32)
    INV = c_(np.concatenate([inv, inv]).reshape(64, 1))
    SGN = c_(np.concatenate([-np.ones(32, np.float32), np.ones(32, np.float32)]).reshape(64, 1))
    MASK = causal_masks()
    QG, KG = gchunk(q_norm), gchunk(kv_norm)
    maps = []
    for r in range(NCORES):
        wq, wkv = [], []
        for h in (2 * r, 2 * r + 1):
            b = h * 192
            wq += [w_q_up[:, b:b + 128], w_q_up[:, b + 128:b + 192], w_q_up[:, b + 128 + ROPE_PERM]]
            wkv += [w_kv_up[:, h * 256:(h + 1) * 256]]
        maps.append({"CT": CT, "QG": QG, "KG": KG, "WQ": c_(np.concatenate(wq, axis=1)),
                     "WKV": c_(np.concatenate(wkv, axis=1)), "POS": POS, "INV": INV, "SGN": SGN, "MASK": MASK})
    outs = launch(ncB, maps)
    OT_full = np.concatenate([o["OTh"] for o in outs], axis=0)
    DBG["OT"] = OT_full
    HMs, XN2s = run_D(OT_full, HTs, w_o, g[1], g[2])
    DBG["HM"] = HMs
    DBG["XN2"] = XN2s
    return run_E(XN2s, HMs, *ffn, g[3])


def rel_thresholds():
    n = np.arange(0, 512)
    nf = np.maximum(n, 1).astype(np.float32)
    large = 16 + (np.log(nf / np.float32(16)) / np.float32(math.log(128 / 16)) * np.float32(16)).astype(np.int32)
    large = np.minimum(large, 31)
    bucket = np.where(n < 16, n, large)
    return [int(np.min(n[bucket >= b])) for b in range(1, 32)]


def emit_C(P, QT, KT, VT, RB, POSQ, POSK, MASK, EALL, IDENT, OTh, ones, b_ones):
    thr = rel_thresholds()
    mask = P.sbuf("C_mask", [128, 4, 512], BF16)
    eall = P.sbuf("C_eall", [32, 32 * 128], BF16)
    ident = P.sbuf("C_ident", [128, 128], F32)
    rb = P.sbuf("C_rb", [128, 2, 32], F32)
    b_mask, b_eall, b_ident, b_rb = Buf(), Buf(), Buf(), Buf()
    DMA(P, "sp", mask[:], MASK, [], [b_mask], b_mask)
    DMA(P, "sp", eall[:], EALL, [], [b_eall], b_eall)
    DMA(P, "sp", ident[:], IDENT, [], [b_ident], b_ident)
    DMA(P, "sp", rb[:], RB, [], [b_rb], b_rb)
    MULT = P.sbuf("C_mult", [128, 2, 5, 512], BF16)
    b_mult = Buf()
    P.push_scope()
    dl = P.sbuf("C_dl", [128, 2, 32], F32)
    c0 = P.sbuf("C_c0", [128, 2], F32)
    b_dl, b_c0 = Buf(), Buf()
    TT(P, "dve", dl[:, :, 1:32], rb[:, :, 1:32], rb[:, :, 0:31], ALU.subtract, [b_rb], [b_dl])
    TT(P, "dve", c0[:], rb[:, :, 0], rb[:, :, 31], ALU.subtract, [b_rb], [b_c0])
    pqi = P.sbuf("C_pqi", [128, 512], I32)
    pki = P.sbuf("C_pki", [128, 5], I32)
    pq = P.sbuf("C_pq", [128, 512], F32)
    pk = P.sbuf("C_pk", [128, 5], F32)
    b_pqi, b_pki, b_pq, b_pk = Buf(), Buf(), Buf(), Buf()
    DMA(P, "sp", pqi[:], POSQ, [], [b_pqi], b_pqi)
    DMA(P, "sp", pki[:], POSK, [], [b_pki], b_pki)
    CP(P, "dve", pq[:], pqi[:], [b_pqi], [b_pq])
    CP(P, "dve", pk[:], pki[:], [b_pki], [b_pk])
    dists = Slots(P, "C_dist", 2, [128, 512], F32)
    accs = Slots(P, "C_acc", 2, [128, 512], F32)
    tmps = Slots(P, "C_tmp", 3, [128, 512], F32)
    for j in range(5):
        dist, bd = dists.next()
        TS(P, "dve", dist[:], pq[:], pk[:, j:j + 1], None, ALU.subtract, None, [b_pq, b_pk], [bd])
        for h in range(2):
            acc, ba = accs.next()
            TS(P, "dve", acc[:], dist[:], float(thr[0]), dl[:, h, 1:2], ALU.is_ge, ALU.mult, [bd, b_dl], [ba])
            for b in range(2, 32):
                tmp, bt = tmps.next()
                TS(P, "dve", tmp[:], dist[:], float(thr[b - 1]), dl[:, h, b:b + 1], ALU.is_ge, ALU.mult, [bd, b_dl], [bt])
                TT(P, "pool", acc[:], acc[:], tmp[:], ALU.add, [ba, bt], [ba])
            if j == 0:
                ACT(P, MULT[:, h, j, :], acc[:], AF.Exp, [ba, b_c0], [b_mult], bias=c0[:, h:h + 1], scale=1.0)
            else:
                ACT(P, acc[:], acc[:], AF.Exp, [ba, b_c0], [ba], bias=c0[:, h:h + 1], scale=1.0)
                TT(P, "pool", MULT[:, h, j, :], acc[:], mask[:, j - 1, :], ALU.mult, [ba, b_mask], [b_mult])
    P.pop_scope()
    Q = P.sbuf("C_Q", [128, S], BF16)
    K = P.sbuf("C_K", [128, S], BF16)
    V = P.sbuf("C_V", [128, 64, 128], BF16)
    MBT = P.sbuf("C_MBT", [32, S], BF16)
    bQ = [Buf() for _ in range(16)]
    bK = [Buf() for _ in range(16)]
    bV = [Buf() for _ in range(16)]
    bMB = [Buf() for _ in range(16)]
    kmf = P.sbuf("C_kmf", [128, 32], F32)
    kmT = P.sbuf("C_kmT", [128, 32], BF16)
    b_kmf, b_kmT = Buf(), Buf()
    gs = Slots(P, "C_g", 2, [128, 32], F32)
    mxs = Slots(P, "C_mx", 2, [128, 8], F32)
    mbs = Slots(P, "C_mb", 2, [128, 32], F32)
    psS = Slots(P, "C_psS", 4, [128, 512], F32, psum=True)
    psM = psS
    psO = Slots(P, "C_psO", 2, [128, 512], F32, psum=True)
    psSUM = Slots(P, "C_psSUM", 2, [128, 512], F32, psum=True)
    pts = Slots(P, "C_pt", 6, [128, 512], BF16)
    rss = Slots(P, "C_rs", 2, [128, 512], F32)
    ots = Slots(P, "C_ot", 2, [128, 512], BF16)
    VTv = VT.rearrange("(t p) d -> p t d", p=128)
    scale = float(128 ** -0.5)
    for hh in range(2):
        for tt in range(16):
            cols = slice(tt * 512, (tt + 1) * 512)
            DMA(P, "sp", Q[:, cols], QT[hh * 128:(hh + 1) * 128, cols], [], [bQ[tt]], bQ[tt])
            DMA(P, "sp", K[:, cols], KT[hh * 128:(hh + 1) * 128, cols], [], [bK[tt]], bK[tt])
            DMA(P, "sp", V[:, tt * 4:(tt + 1) * 4, :], VTv[:, tt * 4:(tt + 1) * 4, hh * 128:(hh + 1) * 128], [], [bV[tt]], bV[tt])
        P.op("dve", lambda e: e.tensor_reduce(out=kmf[:], in_=K[:].rearrange("p (n l) -> p n l", l=256), axis=AX.X, op=ALU.add),
             bK, [b_kmf])
        TS(P, "dve", kmT[:], kmf[:], float(1.0 / 256), None, ALU.mult, None, [b_kmf], [b_kmT])
        for i in range(64):
            own = i // 2
            G, bG = psM.next()
            MM(P, G[:, 0:32], Q[:, i * 128:(i + 1) * 128], kmT[:], True, True, [bQ[i // 4], b_kmT], [bG])
            g, bg = gs.next()
            CP(P, "dve", g[:], G[:, 0:32], [bG], [bg])
            MEMSET(P, "dve", g[:, own:32], -1e30, [bg])
            mx, bmx = mxs.next()
            P.op("dve", lambda e, mx=mx, g=g: e.max(out=mx[:], in_=g[:]), [bg], [bmx])
            mb, bmb = mbs.next()
            TS(P, "dve", mb[:], g[:], mx[:, 2:3], -30000.0, ALU.is_lt, ALU.mult, [bg, bmx], [bmb])
            MEMSET(P, "dve", mb[:, own:own + 1], 0.0, [bmb])
            if own + 1 < 32:
                MEMSET(P, "dve", mb[:, own + 1:32], -30000.0, [bmb])
            T, bT = psM.next()
            P.op("pe", lambda e, T=T, mb=mb: e.transpose(T[0:32, 0:128], mb[:], ident[:]), [bmb, b_ident], [bT])
            CP(P, "act", MBT[:, i * 128:(i + 1) * 128], T[0:32, 0:128], [bT], [bMB[i // 4]])

        def extra_qk(Sp, bS, qt, kt):
            n = kt // 2
            MM(P, Sp[:], eall[:, n * 128:(n + 1) * 128], MBT[:, qt * 512:(qt + 1) * 512], False, True,
               [b_eall, bMB[qt]], [bS])

        def special(qt, kt, hh=hh):
            j = kt - 4 * qt
            if j >= -1:
                return MULT[:, hh, j + 1, :], [b_mult]
            return None

        attn_loop(P, hh, scale, K, bK, Q, bQ, V, bV, OTh, ones, b_ones, psS, psO, psSUM, pts, rss, ots,
                  extra_qk, special)


def build_C():
    nc = new_nc()
    QT = din(nc, "QT", [256, S], BF16)
    KT = din(nc, "KT", [256, S], BF16)
    VT = din(nc, "VT", [S, 256], BF16)
    RB = din(nc, "RB", [128, 2, 32], F32)
    POSQ = din(nc, "POSQ", [128, 512], I32)
    POSK = din(nc, "POSK", [128, 5], I32)
    MASK = din(nc, "MASK", [128, 4, 512], BF16)
    EALL = din(nc, "EALL", [32, 32 * 128], BF16)
    IDENT = din(nc, "IDENT", [128, 128], F32)
    OTh = dout(nc, "OTh", [256, S], BF16)
    P = Prog(nc)
    ones, b_ones = setup_common(P)
    emit_C(P, QT, KT, VT, RB, POSQ, POSK, MASK, EALL, IDENT, OTh, ones, b_ones)
    P.finish()
    return nc


def run_moba_layer(HTs, positions, g, w_q, w_o, rel_bias, KTf, VTf, ffn):
    QTs = run_A(HTs, g[0], np.asarray(w_q, np.float32), NPBF, BF16)
    QTf = np.concatenate(QTs, axis=1)
    ncC = get_prog("C", build_C)
    pos = np.asarray(positions, np.int32).reshape(S)
    POSQ = c_(np.broadcast_to(pos[512:1024].reshape(1, 512), (128, 512)))
    POSK = c_(pos[384:1024].reshape(5, 128).T)
    MASK = causal_masks()
    EALL = c_(np.repeat(np.eye(32, dtype=np.float32)[:, :, None], 128, axis=2).reshape(32, 32 * 128).astype(NPBF))
    IDENT = np.eye(128, dtype=np.float32)
    rel_bias = np.asarray(rel_bias, np.float32)
    maps = []
    for r in range(NCORES):
        RB = c_(np.broadcast_to(rel_bias[:, 2 * r:2 * r + 2].T.reshape(1, 2, 32), (128, 2, 32)))
        maps.append({"QT": c_(QTf[r * 256:(r + 1) * 256]), "KT": c_(KTf[r * 256:(r + 1) * 256]),
                     "VT": c_(VTf[r * 256:(r + 1) * 256].T), "RB": RB, "POSQ": POSQ, "POSK": POSK,
                     "MASK": MASK, "EALL": EALL, "IDENT": IDENT})
    outs = launch(ncC, maps)
    OT_full = np.concatenate([o["OTh"] for o in outs], axis=0)
    DBG["OTm"] = OT_full
    HMs, XN2s = run_D(OT_full, HTs, w_o, g[1], g[2])
    return run_E(XN2s, HMs, *ffn, g[3])


def kernel(x, positions, norm_gains, a_w_in, a_q_norm, a_w_q_up, a_kv_norm, a_w_kv_up, a_w_o,
           b_kv_norm, b_w_kv, b_w_q, b_w_o, rel_bias, ffn_w_in, ffn_conv_w, ffn_conv_b, ffn_w_out):
    x = np.asarray(x, np.float32)
    HTs = [c_(x[0, r * TOK:(r + 1) * TOK, :].T) for r in range(NCORES)]
    KTf = VTf = None
    for layer in range(4):
        g = np.asarray(norm_gains[layer], np.float32)
        ffn = (np.asarray(ffn_w_in[layer], np.float32), ffn_conv_w[layer], ffn_conv_b[layer],
               np.asarray(ffn_w_out[layer], np.float32))
        if layer < 2:
            HTs = run_mla_layer(HTs, positions, g, a_w_in[layer], a_q_norm[layer], np.asarray(a_w_q_up[layer], np.float32),
                                a_kv_norm[layer], np.asarray(a_w_kv_up[layer], np.float32),
                                np.asarray(a_w_o[layer], np.float32), ffn)
        else:
            if layer == 2:
                KVs = run_A(HTs, b_kv_norm, np.asarray(b_w_kv, np.float32), NPBF, BF16)
                KV = np.concatenate(KVs, axis=1)
                KTf, VTf = KV[:2048], KV[2048:]
            j = layer - 2
            HTs = run_moba_layer(HTs, positions, g, b_w_q[j], np.asarray(b_w_o[j], np.float32), rel_bias, KTf, VTf, ffn)
    out = np.concatenate([h.T for h in HTs], axis=0).reshape(1, S, D).astype(np.float32)
    return out
```
